# Optimizing a Trainium2 kernel written in Bass

```python
import jax, jax.numpy as jnp
from jax import lax
import numpy as np

D_MODEL = 1024
BATCH = 8
SEQ = 4096
DEPTH = 1

MIX_WIDTH = D_MODEL
RET_WIDTH = MIX_WIDTH // 2
MOBA_WIDTH = MIX_WIDTH - RET_WIDTH
HEAD_DIM = 128
RET_HEADS = RET_WIDTH // HEAD_DIM
MOBA_HEADS = MOBA_WIDTH // HEAD_DIM
RET_CHUNK = 128
MOBA_BLOCK = 256
MOBA_TOPK = 3
MOBA_QCHUNK = 32
D_FF = -(-8 * D_MODEL // (3 * 256)) * 256
IN_COLS = 4 * RET_WIDTH + 3 * MOBA_WIDTH
IN_SPLITS = (RET_WIDTH, 2 * RET_WIDTH, 3 * RET_WIDTH, 4 * RET_WIDTH,
             4 * RET_WIDTH + MOBA_WIDTH, 4 * RET_WIDTH + 2 * MOBA_WIDTH)
N_ADA = 6
DEEPNORM_ALPHA = (2 * DEPTH) ** 0.25
DEEPNORM_BETA = (8 * DEPTH) ** -0.25
LN_EPS = 1e-5

kernel_name = "hybrid_retention_moba_adaln_deepnorm"


def layer_norm(x, w=None, b=None):
    xf = x.astype(jnp.float32)
    mu = jnp.mean(xf, axis=-1, keepdims=True)
    var = jnp.mean(jnp.square(xf - mu), axis=-1, keepdims=True)
    y = (xf - mu) * lax.rsqrt(var + LN_EPS)
    if w is not None:
        y = y * w.astype(jnp.float32) + b.astype(jnp.float32)
    return y.astype(x.dtype)


def retention(q, k, v, g):
    B, T, H, d = q.shape
    C = RET_CHUNK
    N = T // C
    lg = jnp.log(1.0 - 2.0 ** (-5.0 - jnp.arange(H, dtype=jnp.float32)))
    pos = jnp.arange(C, dtype=jnp.float32)
    rel = pos[:, None] - pos[None, :]
    intra_decay = jnp.where(rel >= 0, jnp.exp(jnp.maximum(rel, 0.0)[None] * lg[:, None, None]), 0.0)
    q_decay = jnp.exp((pos + 1.0)[None] * lg[:, None])
    k_decay = jnp.exp((C - 1.0 - pos)[None] * lg[:, None])
    chunk_decay = jnp.exp(C * lg)

    def chunks(a):
        return a.reshape(B, N, C, H, d).transpose(0, 3, 1, 2, 4)

    qc, kc, vc = chunks(q), chunks(k * (d ** -0.5)), chunks(v)
    s = jnp.einsum('bhncd,bhnsd->bhncs', qc, kc) * intra_decay[:, None]
    intra = jnp.einsum('bhncs,bhnsd->bhncd', s, vc)
    kv = jnp.einsum('bhnsd,bhnse->nbhde', kc * k_decay[:, None, :, None], vc)

    def step(state, kv_n):
        return chunk_decay[:, None, None] * state + kv_n, state

    _, prev = lax.scan(step, jnp.zeros(kv.shape[1:], kv.dtype), kv)
    inter = jnp.einsum('bhncd,nbhde->bhnce', qc * q_decay[:, None, :, None], prev)
    y = (intra + inter).transpose(0, 2, 3, 1, 4).reshape(B, T, H, d)
    y = layer_norm(y).reshape(B, T, H * d)
    return jax.nn.silu(g) * y


def moba_attention(q, k, v):
    B, T, H, d = q.shape
    L = MOBA_BLOCK
    NB = -(-T // L)
    Tp = NB * L
    QC = MOBA_QCHUNK
    K_SEL = min(MOBA_TOPK, NB - 1)
    scale = d ** -0.5
    slopes = 2.0 ** (-8.0 * (jnp.arange(H, dtype=jnp.float32) + 1.0) / H)
    q = q.transpose(0, 2, 1, 3)
    pad = ((0, 0), (0, 0), (0, Tp - T), (0, 0))
    k = jnp.pad(k.transpose(0, 2, 1, 3), pad)
    v = jnp.pad(v.transpose(0, 2, 1, 3), pad)
    k_blocks = k.reshape(B, H, NB, L, d)
    v_blocks = v.reshape(B, H, NB, L, d)
    k_mean = jnp.mean(k_blocks, axis=3)
    bi = jnp.arange(B)[:, None, None, None]
    hi = jnp.arange(H)[None, :, None, None]
    offs = jnp.arange(L)

    def chunk(i):
        t0 = i * QC
        blk = t0 // L
        t = t0 + jnp.arange(QC)
        q_c = lax.dynamic_slice_in_dim(q, t0, QC, axis=2)
        k_own = lax.dynamic_slice_in_dim(k, blk * L, L, axis=2)
        v_own = lax.dynamic_slice_in_dim(v, blk * L, L, axis=2)
        dist_own = (t[:, None] - (blk * L + offs)[None, :]).astype(jnp.float32)
        s_own = (jnp.einsum('bhqd,bhsd->bhqs', q_c, k_own).astype(jnp.float32) * scale
                 - slopes[:, None, None] * dist_own)
        s_own = jnp.where(dist_own >= 0, s_own, -jnp.inf)
        if K_SEL > 0:
            gate = jnp.einsum('bhqd,bhnd->bhqn', q_c, k_mean).astype(jnp.float32)
            gate = jnp.where(jnp.arange(NB) < blk, gate, -jnp.inf)
            _, idx = lax.top_k(gate, K_SEL)
            k_sel = k_blocks[bi, hi, idx]
            v_sel = v_blocks[bi, hi, idx]
            dist_sel = (t[:, None, None] - (idx[..., None] * L + offs)).astype(jnp.float32)
            s_sel = (jnp.einsum('bhqd,bhqksd->bhqks', q_c, k_sel).astype(jnp.float32) * scale
                     - slopes[:, None, None, None] * dist_sel)
            valid = (jnp.arange(K_SEL) < blk)[:, None]
            s_sel = jnp.where(valid, s_sel, -jnp.inf).reshape(B, H, QC, K_SEL * L)
            p = jax.nn.softmax(jnp.concatenate([s_sel, s_own], axis=-1), axis=-1).astype(v.dtype)
            p_sel = p[..., :K_SEL * L].reshape(B, H, QC, K_SEL, L)
            p_own = p[..., K_SEL * L:]
            out = (jnp.einsum('bhqks,bhqksd->bhqd', p_sel, v_sel)
                   + jnp.einsum('bhqs,bhsd->bhqd', p_own, v_own))
        else:
            p_own = jax.nn.softmax(s_own, axis=-1).astype(v.dtype)
            out = jnp.einsum('bhqs,bhsd->bhqd', p_own, v_own)
        return out

    out = lax.map(chunk, jnp.arange(T // QC))
    return out.transpose(1, 0, 3, 2, 4).reshape(B, T, H * d)


def setup_inputs(seed: int = 0) -> dict:
    key = jax.random.key(seed)
    ks = jax.random.split(key, 12)
    f32 = jnp.float32
    x = jax.random.normal(ks[0], (BATCH, SEQ, D_MODEL), f32)
    c = jax.random.normal(ks[1], (BATCH, D_MODEL), f32)
    w_ada = jax.random.normal(ks[2], (DEPTH, D_MODEL, N_ADA * D_MODEL), f32) * D_MODEL ** -0.5
    b_ada = 0.02 * jax.random.normal(ks[3], (DEPTH, N_ADA * D_MODEL), f32)
    col_scale = jnp.concatenate([
        jnp.ones((2 * RET_WIDTH,), f32), jnp.full((RET_WIDTH,), DEEPNORM_BETA, f32),
        jnp.ones((RET_WIDTH + 2 * MOBA_WIDTH,), f32), jnp.full((MOBA_WIDTH,), DEEPNORM_BETA, f32)])
    w_in = jax.random.normal(ks[4], (DEPTH, D_MODEL, IN_COLS), f32) * D_MODEL ** -0.5 * col_scale
    w_out = jax.random.normal(ks[5], (DEPTH, MIX_WIDTH, D_MODEL), f32) * MIX_WIDTH ** -0.5 * DEEPNORM_BETA
    ln1_w = 1.0 + 0.02 * jax.random.normal(ks[6], (DEPTH, D_MODEL), f32)
    ln1_b = 0.02 * jax.random.normal(ks[7], (DEPTH, D_MODEL), f32)
    w_ffn_in = jax.random.normal(ks[8], (DEPTH, D_MODEL, 2 * D_FF), f32) * D_MODEL ** -0.5
    w_ffn_out = jax.random.normal(ks[9], (DEPTH, D_FF, D_MODEL), f32) * D_FF ** -0.5 * DEEPNORM_BETA
    ln2_w = 1.0 + 0.02 * jax.random.normal(ks[10], (DEPTH, D_MODEL), f32)
    ln2_b = 0.02 * jax.random.normal(ks[11], (DEPTH, D_MODEL), f32)
    return {"x": x, "c": c, "w_ada": w_ada, "b_ada": b_ada, "w_in": w_in, "w_out": w_out,
            "ln1_w": ln1_w, "ln1_b": ln1_b, "w_ffn_in": w_ffn_in, "w_ffn_out": w_ffn_out,
            "ln2_w": ln2_w, "ln2_b": ln2_b}


def reference(x, c, w_ada, b_ada, w_in, w_out, ln1_w, ln1_b, w_ffn_in, w_ffn_out, ln2_w, ln2_b):
    B, T, _ = x.shape
    for l in range(DEPTH):
        mod = jax.nn.silu(c) @ w_ada[l] + b_ada[l]
        sh1, sc1, g1, sh2, sc2, g2 = [m[:, None, :] for m in jnp.split(mod, N_ADA, axis=-1)]
        h = layer_norm(x) * (1.0 + sc1) + sh1
        proj = h @ w_in[l]
        rq, rk, rv, rg, mq, mk, mv = jnp.split(proj, IN_SPLITS, axis=-1)
        y_ret = retention(rq.reshape(B, T, RET_HEADS, HEAD_DIM), rk.reshape(B, T, RET_HEADS, HEAD_DIM),
                          rv.reshape(B, T, RET_HEADS, HEAD_DIM), rg)
        y_moba = moba_attention(mq.reshape(B, T, MOBA_HEADS, HEAD_DIM), mk.reshape(B, T, MOBA_HEADS, HEAD_DIM),
                                mv.reshape(B, T, MOBA_HEADS, HEAD_DIM))
        mix = jnp.concatenate([y_ret, y_moba], axis=-1) @ w_out[l]
        x = layer_norm(DEEPNORM_ALPHA * x + g1 * mix, ln1_w[l], ln1_b[l])
        h = layer_norm(x) * (1.0 + sc2) + sh2
        gate, up = jnp.split(h @ w_ffn_in[l], 2, axis=-1)
        ffn = (jax.nn.silu(gate) * up) @ w_ffn_out[l]
        x = layer_norm(DEEPNORM_ALPHA * x + g2 * ffn, ln2_w[l], ln2_b[l])
    return x
```

```python
import contextlib
import math
import numpy as np
import concourse.bass as bass
import concourse.mybir as mybir
from concourse.bass_utils import run_bass_kernel_spmd

F32 = mybir.dt.float32
BF16 = mybir.dt.bfloat16
I32 = mybir.dt.int32
AF = mybir.ActivationFunctionType
ALU = mybir.AluOpType
AX = mybir.AxisListType

T = 4096
D = 1024
P = 128
KC = 8
NT = 32
NG = 8
DFF = 2816
FC = 22
ALPHA = 2.0 ** 0.25
EPS = 1e-5
SCALE = 128.0 ** -0.5
LG = [math.log(1.0 - 2.0 ** (-5.0 - h)) for h in range(4)]
SLOPES = [2.0 ** (-8.0 * (h + 1.0) / 4.0) for h in range(4)]
NEG = -1.0e30

COMPUTE = ("pe", "act", "dve", "pool")
ALL_ENG = ("pe", "act", "dve", "pool", "sp")


class _Op:
    __slots__ = ("eng", "fn", "deps", "sig", "sig_idx", "dma", "dcount", "pos", "bg")

    def __init__(self, eng, fn, dma):
        self.bg = False
        self.eng = eng
        self.fn = fn
        self.deps = []
        self.sig = False
        self.sig_idx = 0
        self.dma = dma
        self.dcount = 0
        self.pos = 0


class Sched:
    def __init__(self, nc):
        self.nc = nc
        self.ops = {e: [] for e in ALL_ENG}
        self.last_w = {}
        self.readers = {}
        self.dma_streams = {}
        self.final_waits = []
        self.dma_since_barrier = []

    def add(self, eng, fn, r=(), w=(), dma=None, extra_deps=(), bg=False):
        op = _Op(eng, fn, dma)
        op.bg = bg
        op.pos = len(self.ops[eng])
        deps = {}

        def add_dep(d, war):
            if d is op:
                return
            if d.dma is None and d.eng == eng:
                if eng == "pe" or war:
                    return
            key = id(d) if d.dma is not None else d.eng
            old = deps.get(key)
            if old is None or (d.dma is None and d.pos > old.pos):
                deps[key] = d

        for k in r:
            lw = self.last_w.get(k)
            if lw is not None:
                add_dep(lw, False)
        for k in w:
            lw = self.last_w.get(k)
            if lw is not None:
                add_dep(lw, False)
            rd = self.readers.get(k)
            if rd:
                for d in rd.values():
                    add_dep(d, True)
        for d in extra_deps:
            key = id(d) if d.dma is not None else d.eng
            old = deps.get(key)
            if old is None or (d.dma is None and d.pos > old.pos):
                if not (d.dma is None and d.eng == eng):
                    deps[key] = d
        op.deps = list(deps.values())
        for k in w:
            self.last_w[k] = op
            self.readers[k] = {}
        for k in r:
            rd = self.readers.setdefault(k, {})
            rd[id(op) if dma is not None else eng] = op
        if dma is not None:
            n = self.dma_streams.get(dma, 0) + 1
            self.dma_streams[dma] = n
            op.dcount = 16 * n
            if not bg:
                self.dma_since_barrier.append(op)
        self.ops[eng].append(op)
        return op

    def barrier(self):
        lasts = [self.ops[e][-1] for e in COMPUTE if self.ops[e]]
        dmas = list(self.dma_since_barrier)
        per = {}
        for d in dmas:
            per[d.dma] = d
        deps = lasts + list(per.values())
        for e in ALL_ENG:
            self.add(e, lambda h: h.nop(), extra_deps=deps)
        self.last_w = {k: v for k, v in self.last_w.items() if v.bg}
        self.readers = {}
        self.dma_since_barrier = []

    def wait_at_end(self, op):
        self.final_waits.append(op)

    def emit(self):
        nc = self.nc
        for e in ALL_ENG:
            for op in self.ops[e]:
                for d in op.deps:
                    if d.dma is None:
                        d.sig = True
        for e in ALL_ENG:
            n = 0
            for op in self.ops[e]:
                if op.sig:
                    n += 1
                    op.sig_idx = n
        with contextlib.ExitStack() as es:
            esem = {e: es.enter_context(nc.semaphore("s_" + e)) for e in ALL_ENG}
            dsem = {name: es.enter_context(nc.semaphore("d_%d" % i))
                    for i, name in enumerate(self.dma_streams)}
            block = es.enter_context(nc.Block())

            def run(e, handle):
                seen = {}
                for op in self.ops[e]:
                    waits = {}
                    for d in op.deps:
                        if d.dma is not None:
                            s, v = dsem[d.dma], d.dcount
                        else:
                            s, v = esem[d.eng], d.sig_idx
                        key = id(s)
                        if key not in waits or waits[key][1] < v:
                            waits[key] = (s, v)
                    for key, (s, v) in waits.items():
                        if seen.get(key, 0) >= v:
                            continue
                        seen[key] = v
                        handle.wait_ge(s, v)
                    ins = op.fn(handle)
                    if op.dma is not None:
                        ins.then_inc(dsem[op.dma], 16)
                    elif op.sig:
                        ins.then_inc(esem[e], 1)
                if e == "sp":
                    for op in self.final_waits:
                        handle.wait_ge(dsem[op.dma], op.dcount)

            @block.tensor
            def _(h):
                run("pe", h)

            @block.scalar
            def _(h):
                run("act", h)

            @block.vector
            def _(h):
                run("dve", h)

            @block.gpsimd
            def _(h):
                run("pool", h)

            @block.sync
            def _(h):
                run("sp", h)


class Arena:
    _n = [0]

    def __init__(self, nc, lo, hi):
        self.nc = nc
        self.LO = lo
        self.HI = hi
        self.top = lo
        self.peak = 0

    def alloc(self, name, shape, dt):
        esz = {F32: 4, BF16: 2, I32: 4}[dt]
        size = esz
        for s in shape[1:]:
            size *= s
        size = (size + 63) // 64 * 64
        Arena._n[0] += 1
        t = self.nc.alloc_sbuf_tensor_at("%s_%d" % (name, Arena._n[0]), list(shape), dt, offset=self.top)
        self.top += size
        self.peak = max(self.peak, self.top)
        assert self.top <= self.HI, "SBUF overflow at %s: %d" % (name, self.top)
        return t

    def mark(self):
        return self.top

    def release(self, m):
        self.top = m


def build_nc():
    nc = bass.Bass("TRN2", target_bir_lowering=False)
    x = nc.dram_tensor("x", [T, D], F32, kind="ExternalInput").ap()
    c = nc.dram_tensor("c", [1, D], F32, kind="ExternalInput").ap()
    w_ada = nc.dram_tensor("w_ada", [D, 6 * D], F32, kind="ExternalInput").ap()
    b_ada = nc.dram_tensor("b_ada", [1, 6 * D], F32, kind="ExternalInput").ap()
    w_in = nc.dram_tensor("w_in", [D, 3584], F32, kind="ExternalInput").ap()
    w_out = nc.dram_tensor("w_out", [D, D], F32, kind="ExternalInput").ap()
    ln1_w = nc.dram_tensor("ln1_w", [1, D], F32, kind="ExternalInput").ap()
    ln1_b = nc.dram_tensor("ln1_b", [1, D], F32, kind="ExternalInput").ap()
    w_ffn_in = nc.dram_tensor("w_ffn_in", [D, 2 * DFF], F32, kind="ExternalInput").ap()
    w_ffn_out = nc.dram_tensor("w_ffn_out", [DFF, D], F32, kind="ExternalInput").ap()
    ln2_w = nc.dram_tensor("ln2_w", [1, D], F32, kind="ExternalInput").ap()
    ln2_b = nc.dram_tensor("ln2_b", [1, D], F32, kind="ExternalInput").ap()
    y = nc.dram_tensor("y", [T, D], F32, kind="ExternalOutput").ap()
    wfi_s = nc.dram_tensor("wfi_s", [D, 2 * DFF], BF16, kind="Internal").ap()
    wfo_s = nc.dram_tensor("wfo_s", [DFF, D], BF16, kind="Internal").ap()
    wfi_r = nc.dram_tensor("wfi_r", [FC, P, KC * 256], BF16, kind="Internal").ap()

    S = Sched(nc)
    SB_LO, SB_HI = 16512, 229376
    o_c = SB_LO
    o_yTr = o_c + 4096
    o_yTm = o_yTr + 32768
    o_hT = o_yTm + 32768
    o_tail = o_hT + 65536
    AC = Arena(nc, o_c, o_yTr)
    A = Arena(nc, o_tail, SB_HI)
    A_yTm = Arena(nc, o_yTm, o_hT)
    A_hT = Arena(nc, o_hT, o_tail)
    pb = [nc.alloc_psum_tensor("pb%d" % i, [P, 512], F32) for i in range(8)]
    pbk = ["pb%d" % i for i in range(8)]

    def pbf(i):
        return pb[i][:].bitcast(BF16)

    def mm(out, lhsT, rhs, start, stop, r, w):
        return S.add("pe", lambda e: e.matmul(out, lhsT=lhsT, rhs=rhs, start=start, stop=stop), r=r, w=w)

    def tr(out, in_, ident, r, w):
        return S.add("pe", lambda e: e.transpose(out=out, in_=in_, identity=ident), r=r, w=w)

    def act(out, in_, func, r, w, bias=None, scale=None):
        kw = {}
        if bias is not None:
            kw["bias"] = bias
        if scale is not None:
            kw["scale"] = scale
        return S.add("act", lambda e: e.activation(out=out, in_=in_, func=func, **kw), r=r, w=w)

    def ts(eng, out, in0, s1, s2, op0, op1, r, w):
        if op1 is None:
            return S.add(eng, lambda e: e.tensor_scalar(out=out, in0=in0, scalar1=s1, scalar2=None, op0=op0), r=r, w=w)
        return S.add(eng, lambda e: e.tensor_scalar(out=out, in0=in0, scalar1=s1, scalar2=s2, op0=op0, op1=op1), r=r, w=w)

    def tt(eng, out, in0, in1, op, r, w):
        return S.add(eng, lambda e: e.tensor_tensor(out=out, in0=in0, in1=in1, op=op), r=r, w=w)

    def stt(out, in0, scalar, in1, op0, op1, r, w):
        return S.add("dve", lambda e: e.scalar_tensor_tensor(out=out, in0=in0, scalar=scalar, in1=in1, op0=op0, op1=op1), r=r, w=w)

    def cp(eng, out, in_, r, w):
        return S.add(eng, lambda e: e.tensor_copy(out=out, in_=in_), r=r, w=w)

    def ms(eng, ap, val, w):
        return S.add(eng, lambda e: e.memset(ap, val), w=w)

    def dma(eng, out, in_, r, w, stream):
        return S.add(eng, lambda e: e.dma_start(out=out, in_=in_), r=r, w=w, dma=stream)

    def iota(out, pattern, base, cm, w):
        return S.add("pool", lambda e: e.iota(out, pattern=pattern, base=base, channel_multiplier=cm), w=w)

    def asel(out, in_, pattern, op, fill, base, cm, r, w):
        return S.add("pool", lambda e: e.affine_select(out=out, in_=in_, pattern=pattern, compare_op=op, fill=fill,
                                                       base=base, channel_multiplier=cm), r=r, w=w)

    ident_f = AC.alloc("identf", [P, P], F32)
    ident_b = AC.alloc("identb", [P, P], BF16)
    ones_f = AC.alloc("onesf", [P, P], F32)
    cmask_f = AC.alloc("cmaskf", [P, P], F32)
    cmask_b = AC.alloc("cmaskb", [P, P], BF16)
    nmask_b = AC.alloc("nmaskb", [P, P], BF16)
    nhalf = AC.alloc("nhalf", [P, 8], F32)
    modT = AC.alloc("modT", [P, 48], F32)
    lnsc = AC.alloc("lnsc", [P, 1], F32)
    ms("pool", ones_f[:], 1.0, ["onesf"])
    ms("pool", ident_f[:], 1.0, ["identf"])
    asel(ident_f[:], ident_f[:], [[-1, P]], ALU.is_equal, 0.0, 0, 1, ["identf"], ["identf"])
    cp("dve", ident_b[:], ident_f[:], ["identf"], ["identb"])
    ms("pool", cmask_f[:], 1.0, ["cmaskf"])
    asel(cmask_f[:], cmask_f[:], [[1, P]], ALU.is_ge, 0.0, 0, -1, ["cmaskf"], ["cmaskf"])
    cp("dve", cmask_b[:], cmask_f[:], ["cmaskf"], ["cmaskb"])
    ts("dve", nmask_b[:], cmask_f[:], -1.0, 30000.0, ALU.add, ALU.mult, ["cmaskf"], ["nmaskb"])
    ms("pool", nhalf[:], -0.5, ["nhalf"])
    ms("pool", lnsc[:], math.log(SCALE), ["lnsc"])
    one11 = ones_f[0:1, 0:1]

    hT = nc.alloc_sbuf_tensor_at("hT", [P, KC, T], BF16, offset=o_hT)
    yTr = nc.alloc_sbuf_tensor_at("yTr", [P, 4, T], BF16, offset=o_yTr)
    yTm = nc.alloc_sbuf_tensor_at("yTm", [P, 4, T], BF16, offset=o_yTm)

    cast_jobs = []
    for k in range(KC):
        for hf in range(2):
            cast_jobs.append((wfi_s[k * P:(k + 1) * P, hf * DFF:(hf + 1) * DFF], w_ffn_in[k * P:(k + 1) * P, hf * DFF:(hf + 1) * DFF], "wfis"))
    for f0 in range(0, FC, 2):
        cast_jobs.append((wfo_s[f0 * P:(f0 + 2) * P, :], w_ffn_out[f0 * P:(f0 + 2) * P, :], "wfos"))

    def cast_ffn_piece(j):
        if j < len(cast_jobs):
            o_, i_, key_ = cast_jobs[j]
            S.add("pool", lambda e: e.dma_start(out=o_, in_=i_), w=[key_], dma=key_, bg=True)
    wfi_sv = wfi_s.rearrange("(k p) n -> p k n", p=P)

    w_in_v = w_in.rearrange("(k p) n -> p k n", p=P)
    Wr = A_yTm.alloc("Wr", [P, KC, 2048], BF16)
    for cbk in range(4):
        dma("pool", Wr[:, :, cbk * 512:(cbk + 1) * 512], w_in_v[:, :, cbk * 512:(cbk + 1) * 512], [], [("Wr", cbk)], "Wr%d" % cbk)

    m0 = A.mark()
    NB0 = 24
    CW = 256
    c_row = A.alloc("crow", [1, D], F32)
    bada = [A.alloc("bada%d" % s, [1, CW], F32) for s in range(2)]
    modrow = A.alloc("modrow", [1, 6 * D], F32)
    scT = A.alloc("scT", [P, KC], F32)
    screp = A.alloc("screp", [P, KC, P], BF16)
    wa = [A.alloc("wa%d" % s, [P, KC, CW], F32) for s in range(2)]
    wab = [A.alloc("wab%d" % s, [P, KC, CW], BF16) for s in range(2)]
    dma("sp", c_row[:], c, [], ["crow"], "crow")
    for k in range(KC):
        mm(pb[0][:, k:k + 1], c_row[0:1, k * P:(k + 1) * P], one11, True, True, ["crow", "onesf"], [pbk[0]])
    act(scT[:], pb[0][:, 0:KC], AF.Silu, [pbk[0]], ["scT"])
    for k in range(KC):
        ts("dve", screp[:, k, :], ones_f[:], scT[:, k:k + 1], None, ALU.mult, None, ["onesf", "scT"], [("screp", k)])
    wada_v = w_ada.rearrange("(k p) n -> p k n", p=P)
    for blk in range(NB0):
        s = blk % 2
        bk = 2 + (blk % 2)
        dma("sp", bada[s][:], b_ada[0:1, blk * CW:(blk + 1) * CW], [], ["bada%d" % s], "bada%d" % s)
        dma("sp", wa[s][:], wada_v[:, :, blk * CW:(blk + 1) * CW], [], ["wa%d" % s], "wa%d" % s)
        if blk % 2 == 0:
            cp("dve", wab[s][:].rearrange("p k n -> p (k n)"), wa[s][:].rearrange("p k n -> p (k n)"), ["wa%d" % s], ["wab%d" % s])
        else:
            act(wab[s][:].rearrange("p k n -> p (k n)"), wa[s][:].rearrange("p k n -> p (k n)"), AF.Identity, ["wa%d" % s], ["wab%d" % s])
        for k in range(KC):
            mm(pb[bk][:, 0:CW], screp[:, k, :], wab[s][:, k, :], k == 0, k == KC - 1, ["wab%d" % s, ("screp", k)], [pbk[bk]])
        tt("dve", modrow[0:1, blk * CW:(blk + 1) * CW], pb[bk][0:1, 0:CW], bada[s][0:1, :], ALU.add,
           [pbk[bk], "bada%d" % s], ["modrow"])
    for j in range(48):
        mm(pb[1][:, j:j + 1], modrow[0:1, j * P:(j + 1) * P], one11, True, True, ["modrow", "onesf"], [pbk[1]])
    cp("dve", modT[:], pb[1][:, 0:48], [pbk[1]], ["modT"])
    ts("dve", modT[:, 8:16], modT[:, 8:16], 1.0, None, ALU.add, None, ["modT"], ["modT"])
    ts("dve", modT[:, 32:40], modT[:, 32:40], 1.0, None, ALU.add, None, ["modT"], ["modT"])
    S.barrier()
    A.release(m0)

    def ln_stats(src, srckey, bufs):
        kp = bufs["key"]
        for cc in range(2):
            S.add("dve", lambda e, cc=cc: e.bn_stats(out=bufs["st"][:, cc, :], in_=src[:, cc * 512:(cc + 1) * 512]),
                  r=[srckey], w=[(kp, "st", cc)])
        S.add("dve", lambda e: e.bn_aggr(out=bufs["mv"][:], in_=bufs["st"][:].rearrange("p a b -> p (a b)")),
              r=[(kp, "st", 0), (kp, "st", 1)], w=[(kp, "mv")])
        ts("dve", bufs["ve"][:], bufs["mv"][:, 1:2], EPS, None, ALU.add, None, [(kp, "mv")], [(kp, "ve")])
        tt("pool", bufs["rs"][:], bufs["ve"][:], nhalf[:, 0:1], ALU.pow, [(kp, "ve"), "nhalf"], [(kp, "rs")])

    def ln_bufs(name):
        return {"st": A.alloc(name + "st", [P, 2, 6], F32), "mv": A.alloc(name + "mv", [P, 2], F32),
                "ve": A.alloc(name + "ve", [P, 1], F32), "rs": A.alloc(name + "rs", [P, 1], F32),
                "nm": A.alloc(name + "nm", [P, 1], F32), "key": name}

    m2 = A.mark()
    xt = [A.alloc("xt%d" % s, [P, D], F32) for s in range(2)]
    xn = [A.alloc("xn%d" % s, [P, D], BF16) for s in range(2)]
    lb = [ln_bufs("l1_%d" % s) for s in range(2)]
    pidx_i = A.alloc("pidxi", [P, 2], I32)
    pidx_f = A.alloc("pidxf", [P, 2], F32)
    qi_i = A.alloc("qii", [P, P], I32)
    qi_f = A.alloc("qif", [P, P], F32)
    A4 = A.alloc("A4", [P, 4], F32)
    KD = A.alloc("KD", [P, 4], F32)
    DTp = A.alloc("DTp", [P, 4, P], F32)
    QD = A.alloc("QD", [P, 4, P], F32)
    iota(pidx_i[:, 0:1], [[0, 1]], 1, 1, [("pidxi", 0)])
    iota(pidx_i[:, 1:2], [[0, 1]], 127, -1, [("pidxi", 1)])
    cp("dve", pidx_f[:], pidx_i[:], [("pidxi", 0), ("pidxi", 1)], ["pidxf"])
    iota(qi_i[:], [[1, P]], 1, 0, ["qii"])
    cp("dve", qi_f[:], qi_i[:], ["qii"], ["qif"])
    for h in range(4):
        act(A4[:, h:h + 1], pidx_f[:, 0:1], AF.Exp, ["pidxf", "lnsc"], [("A4", h)], bias=lnsc[:, 0:1], scale=-LG[h])
        act(KD[:, h:h + 1], pidx_f[:, 1:2], AF.Exp, ["pidxf", "lnsc"], [("KD", h)], bias=lnsc[:, 0:1], scale=LG[h])
        act(QD[:, h, :], qi_f[:], AF.Exp, ["qif"], [("QD", h)], scale=LG[h])
        ts("dve", DTp[:, h, :], cmask_f[:], A4[:, h:h + 1], None, ALU.mult, None, ["cmaskf", ("A4", h)], [("DTp", h)])
    CD = [math.exp(128.0 * LG[h]) for h in range(4)]

    rqdT = [A.alloc("rqdT%d" % s, [P, 4, 512], BF16) for s in range(2)]
    rkT = [A.alloc("rkT%d" % s, [P, 4, 512], BF16) for s in range(2)]
    rkd = [A.alloc("rkd%d" % s, [P, 4, 4, P], BF16) for s in range(2)]
    rv = [A.alloc("rv%d" % s, [P, 4, 512], BF16) for s in range(2)]
    rg1 = A.alloc("rg", [P, 4, 512], F32)
    PT = [A.alloc("PT%d" % s, [P, 4, P], BF16) for s in range(2)]
    ytmp = [A.alloc("ytmp%d" % s, [P, 4, P], F32) for s in range(2)]
    ygate = [A.alloc("ygate%d" % s, [P, 512], BF16) for s in range(2)]
    S32 = A.alloc("S32", [P, 4, P], F32)
    Sbf = [A.alloc("Sbf%d" % s, [P, 4, P], BF16) for s in range(2)]
    gst = [A.alloc("gst%d" % s, [P, 4, 6], F32) for s in range(2)]
    gmv = [A.alloc("gmv%d" % s, [P, 4, 2], F32) for s in range(2)]
    gve = [A.alloc("gve%d" % s, [P, 4], F32) for s in range(2)]
    grs = [A.alloc("grs%d" % s, [P, 4], F32) for s in range(2)]
    gnm = [A.alloc("gnm%d" % s, [P, 4], F32) for s in range(2)]

    def ln_part1(i):
        s2 = i % 2
        dma("sp", xt[s2][:], x[i * P:(i + 1) * P, :], [], ["xt%d" % s2], "xt%d" % s2)
        ln_stats(xt[s2], "xt%d" % s2, lb[s2])
        ts("dve", xn[s2][:], xt[s2][:], lb[s2]["mv"][:, 0:1], lb[s2]["rs"][:, 0:1], ALU.subtract, ALU.mult,
           ["xt%d" % s2, (lb[s2]["key"], "mv"), (lb[s2]["key"], "rs")], ["xn%d" % s2])

    def ln_part2(i):
        s2 = i % 2
        pve = pbf(6)[:, 0:512].rearrange("p (k t) -> p k t", k=4)
        pvo = pbf(7)[:, 512:1024].rearrange("p (k t) -> p k t", k=4)
        for k in range(KC):
            if k % 2 == 0:
                tr(pve[:, k // 2, :], xn[s2][:, k * P:(k + 1) * P], ident_b[:], ["xn%d" % s2, "identb"], [pbk[6]])
            else:
                tr(pvo[:, k // 2, :], xn[s2][:, k * P:(k + 1) * P], ident_b[:], ["xn%d" % s2, "identb"], [pbk[7]])
        for k in range(KC):
            dst = hT[:, k, i * P:(i + 1) * P]
            if k % 2 == 0:
                act(dst, pve[:, k // 2, :], AF.Identity, [pbk[6], "modT"], [("hT", i, k)],
                    bias=modT[:, k:k + 1], scale=modT[:, 8 + k:9 + k])
            else:
                ts("dve", dst, pvo[:, k // 2, :], modT[:, 8 + k:9 + k], modT[:, k:k + 1], ALU.mult, ALU.add,
                   [pbk[7], "modT"], [("hT", i, k)])

    def ln_tile(i):
        ln_part1(i)
        ln_part2(i)

    ucnt = {"fm": 0, "tm": 0}

    def proj_unit(g, u):
        gp = g % 2
        if u < 8:
            cb = u
            bk = ucnt["fm"] % 2
            ucnt["fm"] += 1
            for k in range(KC):
                mm(pb[bk][:], Wr[:, k, cb * P:(cb + 1) * P], hT[:, k, g * 512:(g + 1) * 512], k == 0, k == KC - 1,
                   [("Wr", cb // 4)] + [("hT", 4 * g + ti, k) for ti in range(4)], [pbk[bk]])
            if cb < 4:
                for c4 in range(4):
                    tt("dve", rqdT[gp][:, cb, c4 * P:(c4 + 1) * P], pb[bk][:, c4 * P:(c4 + 1) * P], QD[:, cb, :], ALU.mult,
                       [pbk[bk], ("QD", cb)], [("rqdT", gp, cb)])
            else:
                act(rkT[gp][:, cb - 4, :], pb[bk][:], AF.Identity, [pbk[bk]], [("rkT", gp, cb - 4)])
        else:
            ti, b3 = divmod(u - 8, 3)
            i = 4 * g + ti
            bk = 2 + (ucnt["tm"] % 2)
            ucnt["tm"] += 1
            off = 512 * (b3 + 1)
            for k in range(KC):
                mm(pb[bk][:], hT[:, k, i * P:(i + 1) * P], Wr[:, k, off:off + 512], k == 0, k == KC - 1,
                   [("Wr", b3 + 1), ("hT", i, k)], [pbk[bk]])
            if b3 == 0:
                for h in range(4):
                    act(rkd[gp][:, ti, h, :], pb[bk][:, h * P:(h + 1) * P], AF.Identity, [pbk[bk], ("KD", h)],
                        [("rkd", gp, ti, h)], scale=KD[:, h:h + 1])
            elif b3 == 1:
                cp("dve", rv[gp][:, ti, :], pb[bk][:], [pbk[bk]], [("rv", gp, ti)])
            else:
                act(rg1[:, ti, :], pb[bk][:], AF.Silu, [pbk[bk]], [("rg", ti)])

    def chunk_head(n):
        g, ti = divmod(n, 4)
        gp = g % 2
        cs = n % 2
        pS = pb[4][:].rearrange("p (h c) -> p h c", h=4)
        pY = pb[5][:].rearrange("p (h c) -> p h c", h=4)
        sl = slice(ti * P, (ti + 1) * P)
        for h in range(4):
            mm(pS[:, h, :], rkT[gp][:, h, sl], rqdT[gp][:, h, sl], True, True,
               [("rkT", gp, h), ("rqdT", gp, h)], [pbk[4]])
        tt("dve", PT[cs][:].rearrange("p h c -> p (h c)"), pb[4][:], DTp[:].rearrange("p h c -> p (h c)"), ALU.mult,
           [pbk[4]] + [("DTp", h) for h in range(4)], [("PT", cs)])
        yield
        for h in range(4):
            mm(pY[:, h, :], PT[cs][:, h, :], rv[gp][:, ti, h * P:(h + 1) * P], True, n == 0,
               [("PT", cs), ("rv", gp, ti)], [pbk[5]])
            if n > 0:
                mm(pY[:, h, :], rqdT[gp][:, h, sl], Sbf[(n - 1) % 2][:, h, :], False, True,
                   [("rqdT", gp, h), ("Sbf", (n - 1) % 2)], [pbk[5]])
        if n < NT - 1:
            pKV = pS
            for h in range(4):
                mm(pKV[:, h, :], rkd[gp][:, ti, h, :], rv[gp][:, ti, h * P:(h + 1) * P], True, True,
                   [("rkd", gp, ti, h), ("rv", gp, ti)], [pbk[4]])
            if n == 0:
                cp("dve", S32[:].rearrange("p h c -> p (h c)"), pb[4][:], [pbk[4]], ["S32"])
            else:
                for h in range(4):
                    stt(S32[:, h, :], S32[:, h, :], CD[h], pKV[:, h, :], ALU.mult, ALU.add, ["S32", pbk[4]], ["S32"])
            act(Sbf[cs][:].rearrange("p h c -> p (h c)"), S32[:].rearrange("p h c -> p (h c)"), AF.Identity, ["S32"], [("Sbf", cs)])
        for h in range(4):
            S.add("dve", lambda e, h=h, pY=pY, cs=cs: e.bn_stats(out=gst[cs][:, h, :], in_=pY[:, h, :]),
                  r=[pbk[5]], w=[("gst", cs, h)])
        for h in range(4):
            S.add("dve", lambda e, h=h, cs=cs: e.bn_aggr(out=gmv[cs][:, h, :], in_=gst[cs][:, h, :]),
                  r=[("gst", cs, h)], w=[("gmv", cs, h)])
        ts("dve", gve[cs][:], gmv[cs][:, :, 1], EPS, None, ALU.add, None, [("gmv", cs, h) for h in range(4)], [("gve", cs)])
        tt("pool", grs[cs][:], gve[cs][:], nhalf[:, 0:4], ALU.pow, [("gve", cs), "nhalf"], [("grs", cs)])
        yield
        stt(gnm[cs][:], gmv[cs][:, :, 0], -1.0, grs[cs][:], ALU.mult, ALU.mult,
            [("gmv", cs, h) for h in range(4)] + [("grs", cs)], [("gnm", cs)])
        for h in range(4):
            act(ytmp[cs][:, h, :], pY[:, h, :], AF.Identity, [pbk[5], ("grs", cs), ("gnm", cs)], [("ytmp", cs, h)],
                bias=gnm[cs][:, h:h + 1], scale=grs[cs][:, h:h + 1])
        yield

    def tail_gate(n):
        g, ti = divmod(n, 4)
        cs = n % 2
        tt("dve", ygate[cs][:], ytmp[cs][:].rearrange("p h c -> p (h c)"), rg1[:, ti, :], ALU.mult,
           [("ytmp", cs, h) for h in range(4)] + [("rg", ti)], [("ygate", cs)])

    def tail_tr(n):
        cs = n % 2
        pTr = pbf(7)[:, 0:512].rearrange("p (h c) -> p h c", h=4)
        for h in range(4):
            tr(pTr[:, h, :], ygate[cs][:, h * P:(h + 1) * P], ident_b[:], [("ygate", cs), "identb"], [pbk[7]])
        cp("dve", yTr[:, :, n * P:(n + 1) * P], pTr, [pbk[7]], [("yTr", n)])

    LNA = 9
    for i in range(4):
        ln_tile(i)
    for u in range(20):
        proj_unit(0, u)
        if u % 4 == 3:
            ln_tile(4 + u // 4)
    ln_pending = None
    for n in range(NT):
        g, ti = divmod(n, 4)
        units = [(g + 1, u) for u in (2 * ti, 2 * ti + 1, 8 + 3 * ti, 9 + 3 * ti)] if g + 1 < NG else []
        gen = chunk_head(n)
        next(gen)
        for (gg, u) in units[0:2]:
            proj_unit(gg, u)
        next(gen)
        if n > 0:
            tail_gate(n - 1)
        for (gg, u) in units[2:4]:
            proj_unit(gg, u)
        if n > 0:
            tail_tr(n - 1)
            g1_, t1_ = divmod(n - 1, 4)
            if g1_ + 1 < NG:
                proj_unit(g1_ + 1, 10 + 3 * t1_)
        if ln_pending is not None:
            ln_part2(ln_pending)
            ln_pending = None
        next(gen)
        if n + LNA < NT:
            ln_part1(n + LNA)
            ln_pending = n + LNA
        cast_ffn_piece(n)
    tail_gate(NT - 1)
    tail_tr(NT - 1)
    assert len(cast_jobs) <= NT
    S.barrier()
    A.release(m2)

    for f in range(FC):
        for hf in range(2):
            S.add("sp", lambda e, f=f, hf=hf: e.dma_start(
                out=wfi_r[f].rearrange("p (k c) -> p k c", k=KC)[:, :, hf * P:(hf + 1) * P],
                in_=wfi_sv[:, :, hf * DFF + f * P:hf * DFF + (f + 1) * P]), r=["wfis"], w=["wfir"], dma="wfir", bg=True)
    m3 = A.mark()
    Wm = A.alloc("Wm", [P, KC, 1536], BF16)
    for cbk in range(3):
        dma("pool", Wm[:, :, cbk * 512:(cbk + 1) * 512], w_in_v[:, :, 2048 + cbk * 512:2048 + (cbk + 1) * 512], [],
            [("Wm", cbk)], "Wm%d" % cbk)
    qT = A.alloc("qT", [P, T], BF16)
    kT = A.alloc("kT", [P, T], BF16)
    Va = A.alloc("Va", [P, NT, 129], BF16)
    ksum = A.alloc("ksum", [P, 16], F32)
    ksb = A.alloc("ksb", [P, 16], BF16)
    gmask = A.alloc("gmask", [P, NT, 16], F32)
    gsb = A.alloc("gsb", [P, NT, 16], F32)
    m8 = A.alloc("m8", [P, NT, 8], F32)
    thr = A.alloc("thr", [P, NT], F32)
    sel = A.alloc("sel", [P, NT, 16], F32)
    iot_i = A.alloc("ioti", [P, 32], I32)
    IOT = A.alloc("IOT", [P, 32], F32)
    AB = A.alloc("AB", [P, 32], F32)
    acc = [A.alloc("acc%d" % s, [P, 4, 129], F32) for s in range(2)]
    PTm = [A.alloc("PTm%d" % s, [P, 512], BF16) for s in range(6)]
    rinv = [A.alloc("rinv%d" % s, [P, 1], F32) for s in range(4)]
    ymo = [A.alloc("ymo%d" % s, [P, P], BF16) for s in range(4)]

    ms("dve", Va[:, :, 128:129], 1.0, ["Va1"])
    ms("pool", gmask[:], 0.0, ["gmask"])
    asel(gmask[:].rearrange("p i n -> p (i n)"), gmask[:].rearrange("p i n -> p (i n)"), [[1, 16], [0, 2], [-1, 16]],
         ALU.is_ge, NEG, -1, 0, ["gmask"], ["gmask"])
    iota(iot_i[:], [[128, 32]], -28 * 128 - 256, 1, ["ioti"])
    cp("dve", IOT[:], iot_i[:], ["ioti"], ["IOT"])

    def stage4_prologue():
        for e_ in ("act", "dve", "pool", "sp"):
            S.add(e_, lambda h: h.nop(), w=["hTall"])
        Wo = A_hT.alloc("Wo", [P, KC, D], BF16)
        for cbk in range(2):
            dma("pool", Wo[:, :, cbk * 512:(cbk + 1) * 512], w_out.rearrange("(k p) n -> p k n", p=P)[:, :, cbk * 512:(cbk + 1) * 512],
                [], [("Wo", cbk)], "Wo%d" % cbk)
        lnT = A_hT.alloc("lnT", [P, 4, D], F32)
        g2bc = A_hT.alloc("g2bc", [P, D], F32)
        tmp2 = [A_hT.alloc("tmp2_%d" % s, [P, 512], F32) for s in range(2)]
        actT = A_hT.alloc("actT", [P, FC, 512], BF16)
        A_pro = Arena(nc, A_hT.top - FC * 512 * 2, A_hT.top)
        lnrow = A_pro.alloc("lnrow", [1, 1, D], F32)
        g1bc = A_pro.alloc("g1bc", [P, D], F32)
        Gt = [A_pro.alloc("Gt%d" % s, [P, P], F32) for s in range(2)]
        bc = 0
        for q, src in enumerate((ln1_w, ln1_b, ln2_w, ln2_b)):
            dma("sp", lnrow[0:1, 0, :], src, [], ["lnrow"], "lnrow")
            for cbk in range(2):
                bk = bc % 4
                bc += 1
                mm(pb[bk][:], ones_f[0:1, 0:P], lnrow[0:1, 0, cbk * 512:(cbk + 1) * 512], True, True, ["lnrow", "onesf"], [pbk[bk]])
                cp("dve", lnT[:, q, cbk * 512:(cbk + 1) * 512], pb[bk][:], [pbk[bk]], [("lnT", q)])
        for q, (base, dstt) in enumerate(((16, g1bc), (40, g2bc))):
            for cbk in range(2):
                bk = bc % 4
                bc += 1
                for jj in range(4):
                    j = cbk * 4 + jj
                    gs = (q * 8 + j) % 2
                    ts("dve", Gt[gs][:], ones_f[:], modT[:, base + j:base + j + 1], None, ALU.mult, None, ["onesf", "modT"], [("Gt", gs)])
                    mm(pb[bk][:, jj * P:(jj + 1) * P], Gt[gs][:], ident_f[:], True, True, [("Gt", gs), "identf"], [pbk[bk]])
                cp("dve", dstt[:, cbk * 512:(cbk + 1) * 512], pb[bk][:], [pbk[bk]], [("gbc", q)])
        for k in range(KC):
            tt("dve", Wo[:, k, :], Wo[:, k, :], g1bc[:], ALU.mult, [("Wo", 0), ("Wo", 1), ("gbc", 0)], [("Wof", k)])
        return Wo, lnT, g2bc, tmp2, actT

    mst = {"ocnt": 0, "ptc": 0, "trc": 0}
    NPT = 6
    for hm in range(4):
        hkey = lambda g: [("hT", 4 * g + ti, k) for ti in range(4) for k in range(KC)]
        for g in range(NG):
            bk = g % 2
            for k in range(KC):
                mm(pb[bk][:], Wm[:, k, 512 + hm * P:512 + (hm + 1) * P], hT[:, k, g * 512:(g + 1) * 512], k == 0, k == KC - 1,
                   [("Wm", 1), "hTall"], [pbk[bk]])
            act(kT[:, g * 512:(g + 1) * 512], pb[bk][:], AF.Identity, [pbk[bk]], [("kT", g), ("bser", bk)])
            S.add("dve", lambda e, bk=bk, g=g: e.tensor_reduce(out=ksum[:, 2 * g:2 * g + 2],
                                                               in_=pb[bk][:].rearrange("p (b s) -> p b s", b=2),
                                                               axis=AX.X, op=ALU.add),
                  r=[pbk[bk]], w=[("ksum", g), ("bser", bk)])
        cp("dve", ksb[:], ksum[:], [("ksum", g) for g in range(NG)], ["ksb"])
        for g in range(NG):
            bk = g % 2
            for k in range(KC):
                mm(pb[bk][:], Wm[:, k, hm * P:(hm + 1) * P], hT[:, k, g * 512:(g + 1) * 512], k == 0, k == KC - 1,
                   [("Wm", 0), "hTall"], [pbk[bk]])
            act(qT[:, g * 512:(g + 1) * 512], pb[bk][:], AF.Identity, [pbk[bk]], [("qT", g)])
        pg = pb[2][:].rearrange("p (i n) -> p i n", n=16)
        for i in range(NT):
            mm(pg[:, i, :], qT[:, i * P:(i + 1) * P], ksb[:], True, True, [("qT", i // 4), "ksb"], [pbk[2]])
        tt("dve", gsb[:].rearrange("p i n -> p (i n)"), pb[2][:], gmask[:].rearrange("p i n -> p (i n)"), ALU.add,
           [pbk[2], "gmask"], ["gsb"])
        for i in range(2, NT):
            S.add("dve", lambda e, i=i: e.max(out=m8[:, i, :], in_=gsb[:, i, :]), r=["gsb"], w=[("m8", i)])
        ts("dve", thr[:, 2:NT], m8[:, 2:NT, 2], -1.0e29, None, ALU.max, None, [("m8", i) for i in range(2, NT)], ["thr"])
        for i in range(2, NT):
            ts("dve", sel[:, i, :], gsb[:, i, :], thr[:, i:i + 1], None, ALU.is_ge, None, ["gsb", "thr"], [("sel", i)])
        for i in range(NT):
            bk = 6 + (i % 2)
            for k in range(KC):
                mm(pb[bk][:, 0:P], hT[:, k, i * P:(i + 1) * P], Wm[:, k, 1024 + hm * P:1024 + (hm + 1) * P], k == 0, k == KC - 1,
                   [("Wm", 2), "hTall"], [pbk[bk]])
            act(Va[:, i, 0:P], pb[bk][:, 0:P], AF.Identity, [pbk[bk]], [("Va", i)])
        ts("dve", AB[:], IOT[:], SLOPES[hm], None, ALU.mult, None, ["IOT"], ["AB"])
        def phase1(gq, n, st):
            qts = [ti for ti in range(4) if (2 * gq + ti // 2) >= n]
            pts = {}
            for kt in (2 * n, 2 * n + 1):
                qk = [ti for ti in qts if 4 * gq + ti >= kt]
                if not qk:
                    continue
                c0 = min(qk) * P
                sb = (0, 1, 6, 7)[st["ptc"] % 4]
                ptb = PTm[st["ptc"] % NPT]
                pkey = ("PTm", st["ptc"] % NPT)
                st["ptc"] += 1
                pts[kt] = (ptb, pkey, qk)
                diag = kt >= 4 * gq
                mm(pb[sb][:, c0:512], kT[:, kt * P:(kt + 1) * P], qT[:, gq * 512 + c0:(gq + 1) * 512], True, not diag,
                   [("kT", kt // 4), ("qT", gq)], [pbk[sb]])
                if diag:
                    td = kt - 4 * gq
                    mm(pb[sb][:, td * P:(td + 1) * P], ident_b[:], nmask_b[:], False, True, ["identb", "nmaskb"], [pbk[sb]])
                rel = kt - 4 * gq + 28
                act(ptb[:, c0:512], pb[sb][:, c0:512], AF.Exp, [pbk[sb], "AB"], [pkey], bias=AB[:, rel:rel + 1], scale=SCALE)
            return qts, pts

        def phase2(gq, n, qts, pts, st):
            accs = acc[gq % 2]
            ak = gq % 2
            oset = st["ocnt"] % 2
            st["ocnt"] += 1
            banks = (2 + 2 * oset, 3 + 2 * oset)
            for ti in qts:
                bank = banks[ti // 2]
                reg = pb[bank][:, (ti % 2) * 129:(ti % 2) * 129 + 129]
                kts = [kt for kt in (2 * n, 2 * n + 1) if kt in pts and ti in pts[kt][2]]
                for kt in kts:
                    ptb, pkey, _ = pts[kt]
                    mm(reg, ptb[:, ti * P:(ti + 1) * P], Va[:, kt, :], kt == kts[0], kt == kts[-1],
                       [pkey, ("Va", kt), "Va1"], [pbk[bank]])
            for ti in qts:
                qi = 4 * gq + ti
                bank = banks[ti // 2]
                reg = pb[bank][:, (ti % 2) * 129:(ti % 2) * 129 + 129]
                own = (n == qi // 2)
                sc = 1.0 if own else sel[:, qi, n:n + 1]
                rds = [pbk[bank]] + ([] if own else [("sel", qi)])
                if n == 0:
                    ts("dve", accs[:, ti, :], reg, sc, None, ALU.mult, None, rds, [("acc", ak, ti)])
                else:
                    stt(accs[:, ti, :], reg, sc, accs[:, ti, :], ALU.mult, ALU.add, rds + [("acc", ak, ti)], [("acc", ak, ti)])

        def fin_dve(gq):
            accs = acc[gq % 2]
            ak = gq % 2
            for ti in range(4):
                S.add("dve", lambda e, ti=ti, accs=accs: e.reciprocal(out=rinv[ti][:], in_=accs[:, ti, 128:129]),
                      r=[("acc", ak, ti)], w=[("rinv", ti)])
                ts("dve", ymo[ti][:], accs[:, ti, 0:P], rinv[ti][:, 0:1], None, ALU.mult, None,
                   [("acc", ak, ti), ("rinv", ti)], [("ymo", ti)])

        def fin_pe(gq):
            for ti in range(4):
                qi = 4 * gq + ti
                pT_ = pbf(5)[:, 768 + (ti % 2) * P:768 + (ti % 2 + 1) * P]
                tr(pT_, ymo[ti][:], ident_b[:], [("ymo", ti), "identb"], [pbk[5]])
                cp("dve", yTm[:, hm, qi * P:(qi + 1) * P], pT_, [pbk[5]], [("yTm", hm, qi)])

        blocks = [(gq, n) for gq in range(NG) for n in range(2 * gq + 2)]
        pend = None
        fin_pending = None
        for (gq, n) in blocks:
            qts, pts = phase1(gq, n, mst)
            if pend is not None:
                phase2(*pend, mst)
                if fin_pending is not None:
                    fin_pe(fin_pending)
                    fin_pending = None
                if pend[1] == 2 * pend[0] + 1:
                    fin_dve(pend[0])
                    fin_pending = pend[0]
            pend = (gq, n, qts, pts)
        phase2(*pend, mst)
        if fin_pending is not None:
            fin_pe(fin_pending)
        fin_dve(pend[0])
        fin_pe(pend[0])
    S.barrier()
    A.release(m3)

    Wo, lnT, g2bc, tmp2, actT = stage4_prologue()
    S.barrier()
    x1 = [[A.alloc("x1_%d_%d" % (gp, s), [P, D], F32) for s in range(4)] for gp in range(2)]
    xn2 = [A.alloc("xn2_%d" % s, [P, D], BF16) for s in range(4)]
    h2T = A.alloc("h2T", [P, KC, 512], BF16)
    sg = [A.alloc("sg%d" % s, [P, 512], F32) for s in range(2)]
    NWI = 3
    NWO = 4
    wfi = [A.alloc("wfi%d" % s, [P, KC, 256], BF16) for s in range(3)]
    wfo = [A.alloc("wfo%d" % s, [P, D], BF16) for s in range(NWO)]
    lf = [ln_bufs("lf%d" % s) for s in range(4)]
    lc = [ln_bufs("lc%d" % s) for s in range(4)]

    def yT_sl(hh, i):
        if hh < 4:
            return yTr[:, hh, i * P:(i + 1) * P]
        return yTm[:, hh - 4, i * P:(i + 1) * P]

    def ln_finish(buf, key, lb):
        kp = lb["key"]
        stt(lb["nm"][:], lb["mv"][:, 0:1], -1.0, lb["rs"][:], ALU.mult, ALU.mult, [(kp, "mv"), (kp, "rs")], [(kp, "nm")])

    def ln_norm(buf, key, lb):
        kp = lb["key"]
        act(buf[:], buf[:], AF.Identity, [key, (kp, "rs"), (kp, "nm")], [key], bias=lb["nm"][:, 0:1], scale=lb["rs"][:, 0:1])

    def ln_affine(buf, key, wq):
        tt("dve", buf[:], buf[:], lnT[:, wq, :], ALU.mult, [key, ("lnT", wq)], [key])
        tt("dve", buf[:], buf[:], lnT[:, wq + 1, :], ALU.add, [key, ("lnT", wq + 1)], [key])

    def front_a(g):
        gp = g % 2
        for ti in range(4):
            i = 4 * g + ti
            dma("sp", x1[gp][ti][:], x[i * P:(i + 1) * P, :], [], [("x1", gp, ti)], "xld%d_%d" % (gp, ti))
        for ti in range(4):
            i = 4 * g + ti
            banks = (2 * (ti % 2), 2 * (ti % 2) + 1)
            for cbk in range(2):
                for hh in range(8):
                    mm(pb[banks[cbk]][:], yT_sl(hh, i), Wo[:, hh, cbk * 512:(cbk + 1) * 512], hh == 0, hh == 7,
                       [("Wof", hh)], [pbk[banks[cbk]]])
            for cbk in range(2):
                hs = slice(cbk * 512, (cbk + 1) * 512)
                stt(x1[gp][ti][:, hs], x1[gp][ti][:, hs], ALPHA, pb[banks[cbk]][:], ALU.mult, ALU.add,
                    [("x1", gp, ti), pbk[banks[cbk]]], [("x1", gp, ti)])
            ln_stats(x1[gp][ti], ("x1", gp, ti), lf[ti])
        for ti in range(4):
            ln_finish(x1[gp][ti], ("x1", gp, ti), lf[ti])
        for ti in range(4):
            ln_norm(x1[gp][ti], ("x1", gp, ti), lf[ti])
        for ti in range(4):
            ln_affine(x1[gp][ti], ("x1", gp, ti), 0)
            ln_stats(x1[gp][ti], ("x1", gp, ti), lf[ti])
        for ti in range(4):
            ts("dve", xn2[ti][:], x1[gp][ti][:], lf[ti]["mv"][:, 0:1], lf[ti]["rs"][:, 0:1], ALU.subtract, ALU.mult,
               [("x1", gp, ti), (lf[ti]["key"], "mv"), (lf[ti]["key"], "rs")], [("xn2", ti)])

    def front_b(g):
        for ti in range(4):
            bke, bko = 4 + 2 * (ti % 2), 5 + 2 * (ti % 2)
            pve = pbf(bke).rearrange("p (k t) -> p k t", k=KC)
            pvo = pbf(bko).rearrange("p (k t) -> p k t", k=KC)
            for k in range(KC):
                if k % 2 == 0:
                    tr(pve[:, k // 2, :], xn2[ti][:, k * P:(k + 1) * P], ident_b[:], [("xn2", ti), "identb"], [pbk[bke]])
                else:
                    tr(pvo[:, k // 2, :], xn2[ti][:, k * P:(k + 1) * P], ident_b[:], [("xn2", ti), "identb"], [pbk[bko]])
            for k in range(KC):
                dst = h2T[:, k, ti * P:(ti + 1) * P]
                if k % 2 == 0:
                    act(dst, pve[:, k // 2, :], AF.Identity, [pbk[bke], "modT"], [("h2T", ti, k)],
                        bias=modT[:, 24 + k:25 + k], scale=modT[:, 32 + k:33 + k])
                else:
                    ts("dve", dst, pvo[:, k // 2, :], modT[:, 32 + k:33 + k], modT[:, 24 + k:25 + k], ALU.mult, ALU.add,
                       [pbk[bko], "modT"], [("h2T", ti, k)])

    cnt = {"wi": 0, "wo": 0}

    def ffn_in(g, pending):
        for f in range(FC):
            if pending:
                pending.pop(0)()
            sl_ = cnt["wi"] % NWI
            cnt["wi"] += 1
            dma("sp", wfi[sl_][:].rearrange("p k c -> p (k c)"), wfi_r[f], ["wfir"], [("wfi", sl_)], "wfi%d" % sl_)
            gb, ub = 4 + 2 * (f % 2), 5 + 2 * (f % 2)
            for half, bk in ((0, gb), (1, ub)):
                for k in range(KC):
                    mm(pb[bk][:], wfi[sl_][:, k, half * P:(half + 1) * P], h2T[:, k, :], k == 0, k == KC - 1,
                       [("wfi", sl_)] + [("h2T", ti, k) for ti in range(4)], [pbk[bk]])
            act(sg[f % 2][:], pb[gb][:], AF.Silu, [pbk[gb]], [("sg", f % 2)])
            tt("dve", actT[:, f, :], sg[f % 2][:], pb[ub][:], ALU.mult, [("sg", f % 2), pbk[ub]], [("actT", f)])

    def ffn_out(g):
        for f in range(FC):
            sl_ = cnt["wo"] % NWO
            cnt["wo"] += 1
            dma("sp", wfo[sl_][:], wfo_s[f * P:(f + 1) * P, :], ["wfos"], [("wfo", sl_)], "wfo%d" % sl_)
            for ti in range(4):
                for cbk in range(2):
                    bk = ti * 2 + cbk
                    mm(pb[bk][:], actT[:, f, ti * P:(ti + 1) * P], wfo[sl_][:, cbk * 512:(cbk + 1) * 512], f == 0, f == FC - 1,
                       [("actT", f), ("wfo", sl_)], [pbk[bk]])

    def tail_a1(g):
        gp = g % 2
        tc = 0
        for ti in (2, 3, 0, 1):
            for cbk in range(2):
                hs = slice(cbk * 512, (cbk + 1) * 512)
                bk = ti * 2 + cbk
                t2 = tmp2[tc % 2]
                tk = ("tmp2", tc % 2)
                tc += 1
                tt("dve", t2[:], pb[bk][:], g2bc[:, hs], ALU.mult, [pbk[bk], ("gbc", 1)], [tk])
                stt(x1[gp][ti][:, hs], x1[gp][ti][:, hs], ALPHA, t2[:], ALU.mult, ALU.add, [("x1", gp, ti), tk], [("x1", gp, ti)])

    def tail_items(g):
        gp = g % 2
        items = []
        for ti in range(4):
            items.append(lambda ti=ti: ln_stats(x1[gp][ti], ("x1", gp, ti), lc[ti]))
        for ti in range(4):
            items.append(lambda ti=ti: ln_finish(x1[gp][ti], ("x1", gp, ti), lc[ti]))
        for ti in range(4):
            items.append(lambda ti=ti: ln_norm(x1[gp][ti], ("x1", gp, ti), lc[ti]))
        for ti in range(4):
            items.append(lambda ti=ti: tt("dve", x1[gp][ti][:], x1[gp][ti][:], lnT[:, 2, :], ALU.mult,
                                          [("x1", gp, ti), ("lnT", 2)], [("x1", gp, ti)]))
        for ti in range(4):
            def fin(ti=ti):
                tt("dve", x1[gp][ti][:], x1[gp][ti][:], lnT[:, 3, :], ALU.add, [("x1", gp, ti), ("lnT", 3)], [("x1", gp, ti)])
                i = 4 * g + ti
                S.wait_at_end(dma("pool", y[i * P:(i + 1) * P, :], x1[gp][ti][:], [("x1", gp, ti)], [("y", i)], "yout%d_%d" % (gp, ti)))
            items.append(fin)
        return items

    front_a(0)
    front_b(0)
    pending = []
    for g in range(NG):
        ffn_in(g, pending)
        assert not pending
        if g + 1 < NG:
            front_a(g + 1)
        ffn_out(g)
        tail_a1(g)
        if g + 1 < NG:
            front_b(g + 1)
        pending = tail_items(g)
    for it in pending:
        it()
    S.emit()
    return nc


_NC_CACHE = {}


def kernel(x, c, w_ada, b_ada, w_in, w_out, ln1_w, ln1_b, w_ffn_in, w_ffn_out, ln2_w, ln2_b):
    f = lambda a: np.ascontiguousarray(np.asarray(a, dtype=np.float32))
    x = f(x)
    c = f(c)
    B = x.shape[0]
    if "nc" not in _NC_CACHE:
        _NC_CACHE["nc"] = build_nc()
    nc = _NC_CACHE["nc"]
    shared = {
        "w_ada": f(w_ada)[0], "b_ada": f(b_ada)[0:1], "w_in": f(w_in)[0], "w_out": f(w_out)[0],
        "ln1_w": f(ln1_w)[0:1], "ln1_b": f(ln1_b)[0:1], "w_ffn_in": f(w_ffn_in)[0], "w_ffn_out": f(w_ffn_out)[0],
        "ln2_w": f(ln2_w)[0:1], "ln2_b": f(ln2_b)[0:1],
    }
    in_maps = []
    for b in range(B):
        m = dict(shared)
        m["x"] = x[b]
        m["c"] = c[b:b + 1]
        in_maps.append(m)
    res = run_bass_kernel_spmd(nc, in_maps, core_ids=list(range(B)))
    return np.stack([np.asarray(r["y"], dtype=np.float32) for r in res.results], axis=0)
```

```python
import contextlib
import math
import numpy as np
import concourse.bass as bass
import concourse.mybir as mybir
from concourse.bass_utils import run_bass_kernel_spmd

F32 = mybir.dt.float32
BF16 = mybir.dt.bfloat16
I32 = mybir.dt.int32
AF = mybir.ActivationFunctionType
ALU = mybir.AluOpType
AX = mybir.AxisListType

T = 4096
D = 1024
P = 128
KC = 8
NT = 32
NG = 8
DFF = 2816
FC = 22
ALPHA = 2.0 ** 0.25
EPS = 1e-5
SCALE = 128.0 ** -0.5
LG = [math.log(1.0 - 2.0 ** (-5.0 - h)) for h in range(4)]
SLOPES = [2.0 ** (-8.0 * (h + 1.0) / 4.0) for h in range(4)]
NEG = -1.0e30

COMPUTE = ("pe", "act", "dve", "pool")
ALL_ENG = ("pe", "act", "dve", "pool", "sp")


class _Op:
    __slots__ = ("eng", "fn", "deps", "sig", "sig_idx", "dma", "dcount", "pos", "bg")

    def __init__(self, eng, fn, dma):
        self.bg = False
        self.eng = eng
        self.fn = fn
        self.deps = []
        self.sig = False
        self.sig_idx = 0
        self.dma = dma
        self.dcount = 0
        self.pos = 0


class Sched:
    def __init__(self, nc):
        self.nc = nc
        self.ops = {e: [] for e in ALL_ENG}
        self.last_w = {}
        self.readers = {}
        self.dma_streams = {}
        self.final_waits = []
        self.dma_since_barrier = []

    def add(self, eng, fn, r=(), w=(), dma=None, extra_deps=(), bg=False):
        op = _Op(eng, fn, dma)
        op.bg = bg
        op.pos = len(self.ops[eng])
        deps = {}

        def add_dep(d, war):
            if d is op:
                return
            if d.dma is None and d.eng == eng:
                if eng == "pe" or war:
                    return
            key = id(d) if d.dma is not None else d.eng
            old = deps.get(key)
            if old is None or (d.dma is None and d.pos > old.pos):
                deps[key] = d

        for k in r:
            lw = self.last_w.get(k)
            if lw is not None:
                add_dep(lw, False)
        for k in w:
            lw = self.last_w.get(k)
            if lw is not None:
                add_dep(lw, False)
            rd = self.readers.get(k)
            if rd:
                for d in rd.values():
                    add_dep(d, True)
        for d in extra_deps:
            key = id(d) if d.dma is not None else d.eng
            old = deps.get(key)
            if old is None or (d.dma is None and d.pos > old.pos):
                if not (d.dma is None and d.eng == eng):
                    deps[key] = d
        op.deps = list(deps.values())
        for k in w:
            self.last_w[k] = op
            self.readers[k] = {}
        for k in r:
            rd = self.readers.setdefault(k, {})
            rd[id(op) if dma is not None else eng] = op
        if dma is not None:
            n = self.dma_streams.get(dma, 0) + 1
            self.dma_streams[dma] = n
            op.dcount = 16 * n
            if not bg:
                self.dma_since_barrier.append(op)
        self.ops[eng].append(op)
        return op

    def barrier(self):
        lasts = [self.ops[e][-1] for e in COMPUTE if self.ops[e]]
        dmas = list(self.dma_since_barrier)
        per = {}
        for d in dmas:
            per[d.dma] = d
        deps = lasts + list(per.values())
        for e in ALL_ENG:
            self.add(e, lambda h: h.nop(), extra_deps=deps)
        self.last_w = {k: v for k, v in self.last_w.items() if v.bg}
        self.readers = {}
        self.dma_since_barrier = []

    def wait_at_end(self, op):
        self.final_waits.append(op)

    def emit(self):
        nc = self.nc
        for e in ALL_ENG:
            for op in self.ops[e]:
                for d in op.deps:
                    if d.dma is None:
                        d.sig = True
        for e in ALL_ENG:
            n = 0
            for op in self.ops[e]:
                if op.sig:
                    n += 1
                    op.sig_idx = n
        with contextlib.ExitStack() as es:
            esem = {e: es.enter_context(nc.semaphore("s_" + e)) for e in ALL_ENG}
            dsem = {name: es.enter_context(nc.semaphore("d_%d" % i))
                    for i, name in enumerate(self.dma_streams)}
            block = es.enter_context(nc.Block())

            def run(e, handle):
                seen = {}
                for op in self.ops[e]:
                    waits = {}
                    for d in op.deps:
                        if d.dma is not None:
                            s, v = dsem[d.dma], d.dcount
                        else:
                            s, v = esem[d.eng], d.sig_idx
                        key = id(s)
                        if key not in waits or waits[key][1] < v:
                            waits[key] = (s, v)
                    for key, (s, v) in waits.items():
                        if seen.get(key, 0) >= v:
                            continue
                        seen[key] = v
                        handle.wait_ge(s, v)
                    ins = op.fn(handle)
                    if op.dma is not None:
                        ins.then_inc(dsem[op.dma], 16)
                    elif op.sig:
                        ins.then_inc(esem[e], 1)
                if e == "sp":
                    for op in self.final_waits:
                        handle.wait_ge(dsem[op.dma], op.dcount)

            @block.tensor
            def _(h):
                run("pe", h)

            @block.scalar
            def _(h):
                run("act", h)

            @block.vector
            def _(h):
                run("dve", h)

            @block.gpsimd
            def _(h):
                run("pool", h)

            @block.sync
            def _(h):
                run("sp", h)


class Arena:
    _n = [0]

    def __init__(self, nc, lo, hi):
        self.nc = nc
        self.LO = lo
        self.HI = hi
        self.top = lo
        self.peak = 0

    def alloc(self, name, shape, dt):
        esz = {F32: 4, BF16: 2, I32: 4}[dt]
        size = esz
        for s in shape[1:]:
            size *= s
        size = (size + 63) // 64 * 64
        Arena._n[0] += 1
        t = self.nc.alloc_sbuf_tensor_at("%s_%d" % (name, Arena._n[0]), list(shape), dt, offset=self.top)
        self.top += size
        self.peak = max(self.peak, self.top)
        assert self.top <= self.HI, "SBUF overflow at %s: %d" % (name, self.top)
        return t

    def mark(self):
        return self.top

    def release(self, m):
        self.top = m


def build_nc():
    nc = bass.Bass("TRN2", target_bir_lowering=False)
    x = nc.dram_tensor("x", [T, D], F32, kind="ExternalInput").ap()
    c = nc.dram_tensor("c", [1, D], F32, kind="ExternalInput").ap()
    w_ada = nc.dram_tensor("w_ada", [D, 6 * D], F32, kind="ExternalInput").ap()
    b_ada = nc.dram_tensor("b_ada", [1, 6 * D], F32, kind="ExternalInput").ap()
    w_in = nc.dram_tensor("w_in", [D, 3584], F32, kind="ExternalInput").ap()
    w_out = nc.dram_tensor("w_out", [D, D], F32, kind="ExternalInput").ap()
    ln1_w = nc.dram_tensor("ln1_w", [1, D], F32, kind="ExternalInput").ap()
    ln1_b = nc.dram_tensor("ln1_b", [1, D], F32, kind="ExternalInput").ap()
    w_ffn_in = nc.dram_tensor("w_ffn_in", [D, 2 * DFF], F32, kind="ExternalInput").ap()
    w_ffn_out = nc.dram_tensor("w_ffn_out", [DFF, D], F32, kind="ExternalInput").ap()
    ln2_w = nc.dram_tensor("ln2_w", [1, D], F32, kind="ExternalInput").ap()
    ln2_b = nc.dram_tensor("ln2_b", [1, D], F32, kind="ExternalInput").ap()
    y = nc.dram_tensor("y", [T, D], F32, kind="ExternalOutput").ap()
    wfi_s = nc.dram_tensor("wfi_s", [D, 2 * DFF], BF16, kind="Internal").ap()
    wfo_s = nc.dram_tensor("wfo_s", [DFF, D], BF16, kind="Internal").ap()
    wfi_r = nc.dram_tensor("wfi_r", [FC, P, KC * 256], BF16, kind="Internal").ap()

    S = Sched(nc)
    SB_LO, SB_HI = 16512, 229376
    o_c = SB_LO
    o_yTr = o_c + 4096
    o_yTm = o_yTr + 32768
    o_hT = o_yTm + 32768
    o_tail = o_hT + 65536
    AC = Arena(nc, o_c, o_yTr)
    A = Arena(nc, o_tail, SB_HI)
    A_yTm = Arena(nc, o_yTm, o_hT)
    A_hT = Arena(nc, o_hT, o_tail)
    pb = [nc.alloc_psum_tensor("pb%d" % i, [P, 512], F32) for i in range(8)]
    pbk = ["pb%d" % i for i in range(8)]

    def pbf(i):
        return pb[i][:].bitcast(BF16)

    def mm(out, lhsT, rhs, start, stop, r, w):
        return S.add("pe", lambda e: e.matmul(out, lhsT=lhsT, rhs=rhs, start=start, stop=stop), r=r, w=w)

    def tr(out, in_, ident, r, w):
        return S.add("pe", lambda e: e.transpose(out=out, in_=in_, identity=ident), r=r, w=w)

    def act(out, in_, func, r, w, bias=None, scale=None):
        kw = {}
        if bias is not None:
            kw["bias"] = bias
        if scale is not None:
            kw["scale"] = scale
        return S.add("act", lambda e: e.activation(out=out, in_=in_, func=func, **kw), r=r, w=w)

    def ts(eng, out, in0, s1, s2, op0, op1, r, w):
        if op1 is None:
            return S.add(eng, lambda e: e.tensor_scalar(out=out, in0=in0, scalar1=s1, scalar2=None, op0=op0), r=r, w=w)
        return S.add(eng, lambda e: e.tensor_scalar(out=out, in0=in0, scalar1=s1, scalar2=s2, op0=op0, op1=op1), r=r, w=w)

    def tt(eng, out, in0, in1, op, r, w):
        return S.add(eng, lambda e: e.tensor_tensor(out=out, in0=in0, in1=in1, op=op), r=r, w=w)

    def stt(out, in0, scalar, in1, op0, op1, r, w):
        return S.add("dve", lambda e: e.scalar_tensor_tensor(out=out, in0=in0, scalar=scalar, in1=in1, op0=op0, op1=op1), r=r, w=w)

    def cp(eng, out, in_, r, w):
        return S.add(eng, lambda e: e.tensor_copy(out=out, in_=in_), r=r, w=w)

    def ms(eng, ap, val, w):
        return S.add(eng, lambda e: e.memset(ap, val), w=w)

    def dma(eng, out, in_, r, w, stream):
        return S.add(eng, lambda e: e.dma_start(out=out, in_=in_), r=r, w=w, dma=stream)

    def iota(out, pattern, base, cm, w):
        return S.add("pool", lambda e: e.iota(out, pattern=pattern, base=base, channel_multiplier=cm), w=w)

    def asel(out, in_, pattern, op, fill, base, cm, r, w):
        return S.add("pool", lambda e: e.affine_select(out=out, in_=in_, pattern=pattern, compare_op=op, fill=fill,
                                                       base=base, channel_multiplier=cm), r=r, w=w)

    ident_f = AC.alloc("identf", [P, P], F32)
    ident_b = AC.alloc("identb", [P, P], BF16)
    ones_f = AC.alloc("onesf", [P, P], F32)
    cmask_f = AC.alloc("cmaskf", [P, P], F32)
    cmask_b = AC.alloc("cmaskb", [P, P], BF16)
    nmask_b = AC.alloc("nmaskb", [P, P], BF16)
    nhalf = AC.alloc("nhalf", [P, 8], F32)
    modT = AC.alloc("modT", [P, 48], F32)
    lnsc = AC.alloc("lnsc", [P, 1], F32)
    ms("pool", ones_f[:], 1.0, ["onesf"])
    ms("pool", ident_f[:], 1.0, ["identf"])
    asel(ident_f[:], ident_f[:], [[-1, P]], ALU.is_equal, 0.0, 0, 1, ["identf"], ["identf"])
    cp("dve", ident_b[:], ident_f[:], ["identf"], ["identb"])
    ms("pool", cmask_f[:], 1.0, ["cmaskf"])
    asel(cmask_f[:], cmask_f[:], [[1, P]], ALU.is_ge, 0.0, 0, -1, ["cmaskf"], ["cmaskf"])
    cp("dve", cmask_b[:], cmask_f[:], ["cmaskf"], ["cmaskb"])
    ts("dve", nmask_b[:], cmask_f[:], -1.0, 30000.0, ALU.add, ALU.mult, ["cmaskf"], ["nmaskb"])
    ms("pool", nhalf[:], -0.5, ["nhalf"])
    ms("pool", lnsc[:], math.log(SCALE), ["lnsc"])
    one11 = ones_f[0:1, 0:1]

    hT = nc.alloc_sbuf_tensor_at("hT", [P, KC, T], BF16, offset=o_hT)
    yTr = nc.alloc_sbuf_tensor_at("yTr", [P, 4, T], BF16, offset=o_yTr)
    yTm = nc.alloc_sbuf_tensor_at("yTm", [P, 4, T], BF16, offset=o_yTm)

    cast_jobs = []
    for k in range(KC):
        for hf in range(2):
            cast_jobs.append((wfi_s[k * P:(k + 1) * P, hf * DFF:(hf + 1) * DFF], w_ffn_in[k * P:(k + 1) * P, hf * DFF:(hf + 1) * DFF], "wfis"))
    for f0 in range(0, FC, 2):
        cast_jobs.append((wfo_s[f0 * P:(f0 + 2) * P, :], w_ffn_out[f0 * P:(f0 + 2) * P, :], "wfos"))

    def cast_ffn_piece(j):
        if j < len(cast_jobs):
            o_, i_, key_ = cast_jobs[j]
            S.add("pool", lambda e: e.dma_start(out=o_, in_=i_), w=[key_], dma=key_, bg=True)
    wfi_sv = wfi_s.rearrange("(k p) n -> p k n", p=P)

    w_in_v = w_in.rearrange("(k p) n -> p k n", p=P)
    Wr = A_yTm.alloc("Wr", [P, KC, 2048], BF16)
    for cbk in range(4):
        dma("pool", Wr[:, :, cbk * 512:(cbk + 1) * 512], w_in_v[:, :, cbk * 512:(cbk + 1) * 512], [], [("Wr", cbk)], "Wr%d" % cbk)

    m0 = A.mark()
    NB0 = 24
    CW = 256
    c_row = A.alloc("crow", [1, D], F32)
    bada = [A.alloc("bada%d" % s, [1, CW], F32) for s in range(2)]
    modrow = A.alloc("modrow", [1, 6 * D], F32)
    scT = A.alloc("scT", [P, KC], F32)
    screp = A.alloc("screp", [P, KC, P], BF16)
    wa = [A.alloc("wa%d" % s, [P, KC, CW], F32) for s in range(2)]
    wab = [A.alloc("wab%d" % s, [P, KC, CW], BF16) for s in range(2)]
    dma("sp", c_row[:], c, [], ["crow"], "crow")
    for k in range(KC):
        mm(pb[0][:, k:k + 1], c_row[0:1, k * P:(k + 1) * P], one11, True, True, ["crow", "onesf"], [pbk[0]])
    act(scT[:], pb[0][:, 0:KC], AF.Silu, [pbk[0]], ["scT"])
    for k in range(KC):
        ts("dve", screp[:, k, :], ones_f[:], scT[:, k:k + 1], None, ALU.mult, None, ["onesf", "scT"], [("screp", k)])
    wada_v = w_ada.rearrange("(k p) n -> p k n", p=P)
    for blk in range(NB0):
        s = blk % 2
        bk = 2 + (blk % 2)
        dma("sp", bada[s][:], b_ada[0:1, blk * CW:(blk + 1) * CW], [], ["bada%d" % s], "bada%d" % s)
        dma("sp", wa[s][:], wada_v[:, :, blk * CW:(blk + 1) * CW], [], ["wa%d" % s], "wa%d" % s)
        if blk % 2 == 0:
            cp("dve", wab[s][:].rearrange("p k n -> p (k n)"), wa[s][:].rearrange("p k n -> p (k n)"), ["wa%d" % s], ["wab%d" % s])
        else:
            act(wab[s][:].rearrange("p k n -> p (k n)"), wa[s][:].rearrange("p k n -> p (k n)"), AF.Identity, ["wa%d" % s], ["wab%d" % s])
        for k in range(KC):
            mm(pb[bk][:, 0:CW], screp[:, k, :], wab[s][:, k, :], k == 0, k == KC - 1, ["wab%d" % s, ("screp", k)], [pbk[bk]])
        tt("dve", modrow[0:1, blk * CW:(blk + 1) * CW], pb[bk][0:1, 0:CW], bada[s][0:1, :], ALU.add,
           [pbk[bk], "bada%d" % s], ["modrow"])
    for j in range(48):
        mm(pb[1][:, j:j + 1], modrow[0:1, j * P:(j + 1) * P], one11, True, True, ["modrow", "onesf"], [pbk[1]])
    cp("dve", modT[:], pb[1][:, 0:48], [pbk[1]], ["modT"])
    ts("dve", modT[:, 8:16], modT[:, 8:16], 1.0, None, ALU.add, None, ["modT"], ["modT"])
    ts("dve", modT[:, 32:40], modT[:, 32:40], 1.0, None, ALU.add, None, ["modT"], ["modT"])
    S.barrier()
    A.release(m0)

    def ln_stats(src, srckey, bufs):
        kp = bufs["key"]
        for cc in range(2):
            S.add("dve", lambda e, cc=cc: e.bn_stats(out=bufs["st"][:, cc, :], in_=src[:, cc * 512:(cc + 1) * 512]),
                  r=[srckey], w=[(kp, "st", cc)])
        S.add("dve", lambda e: e.bn_aggr(out=bufs["mv"][:], in_=bufs["st"][:].rearrange("p a b -> p (a b)")),
              r=[(kp, "st", 0), (kp, "st", 1)], w=[(kp, "mv")])
        ts("dve", bufs["ve"][:], bufs["mv"][:, 1:2], EPS, None, ALU.add, None, [(kp, "mv")], [(kp, "ve")])
        tt("pool", bufs["rs"][:], bufs["ve"][:], nhalf[:, 0:1], ALU.pow, [(kp, "ve"), "nhalf"], [(kp, "rs")])

    def ln_bufs(name):
        return {"st": A.alloc(name + "st", [P, 2, 6], F32), "mv": A.alloc(name + "mv", [P, 2], F32),
                "ve": A.alloc(name + "ve", [P, 1], F32), "rs": A.alloc(name + "rs", [P, 1], F32),
                "nm": A.alloc(name + "nm", [P, 1], F32), "key": name}

    m2 = A.mark()
    xt = [A.alloc("xt%d" % s, [P, D], F32) for s in range(2)]
    xn = [A.alloc("xn%d" % s, [P, D], BF16) for s in range(2)]
    lb = [ln_bufs("l1_%d" % s) for s in range(2)]
    pidx_i = A.alloc("pidxi", [P, 2], I32)
    pidx_f = A.alloc("pidxf", [P, 2], F32)
    qi_i = A.alloc("qii", [P, P], I32)
    qi_f = A.alloc("qif", [P, P], F32)
    A4 = A.alloc("A4", [P, 4], F32)
    KD = A.alloc("KD", [P, 4], F32)
    DTp = A.alloc("DTp", [P, 4, P], F32)
    QD = A.alloc("QD", [P, 4, P], F32)
    iota(pidx_i[:, 0:1], [[0, 1]], 1, 1, [("pidxi", 0)])
    iota(pidx_i[:, 1:2], [[0, 1]], 127, -1, [("pidxi", 1)])
    cp("dve", pidx_f[:], pidx_i[:], [("pidxi", 0), ("pidxi", 1)], ["pidxf"])
    iota(qi_i[:], [[1, P]], 1, 0, ["qii"])
    cp("dve", qi_f[:], qi_i[:], ["qii"], ["qif"])
    for h in range(4):
        act(A4[:, h:h + 1], pidx_f[:, 0:1], AF.Exp, ["pidxf", "lnsc"], [("A4", h)], bias=lnsc[:, 0:1], scale=-LG[h])
        act(KD[:, h:h + 1], pidx_f[:, 1:2], AF.Exp, ["pidxf", "lnsc"], [("KD", h)], bias=lnsc[:, 0:1], scale=LG[h])
        act(QD[:, h, :], qi_f[:], AF.Exp, ["qif"], [("QD", h)], scale=LG[h])
        ts("dve", DTp[:, h, :], cmask_f[:], A4[:, h:h + 1], None, ALU.mult, None, ["cmaskf", ("A4", h)], [("DTp", h)])
    CD = [math.exp(128.0 * LG[h]) for h in range(4)]

    rqdT = [A.alloc("rqdT%d" % s, [P, 4, 512], BF16) for s in range(2)]
    rkT = [A.alloc("rkT%d" % s, [P, 4, 512], BF16) for s in range(2)]
    rkd = [A.alloc("rkd%d" % s, [P, 4, 4, P], BF16) for s in range(2)]
    rv = [A.alloc("rv%d" % s, [P, 4, 512], BF16) for s in range(2)]
    rg1 = A.alloc("rg", [P, 4, 512], F32)
    PT = [A.alloc("PT%d" % s, [P, 4, P], BF16) for s in range(2)]
    ytmp = [A.alloc("ytmp%d" % s, [P, 4, P], F32) for s in range(2)]
    ygate = [A.alloc("ygate%d" % s, [P, 512], BF16) for s in range(2)]
    S32 = A.alloc("S32", [P, 4, P], F32)
    Sbf = [A.alloc("Sbf%d" % s, [P, 4, P], BF16) for s in range(2)]
    gst = [A.alloc("gst%d" % s, [P, 4, 6], F32) for s in range(2)]
    gmv = [A.alloc("gmv%d" % s, [P, 4, 2], F32) for s in range(2)]
    gve = [A.alloc("gve%d" % s, [P, 4], F32) for s in range(2)]
    grs = [A.alloc("grs%d" % s, [P, 4], F32) for s in range(2)]
    gnm = [A.alloc("gnm%d" % s, [P, 4], F32) for s in range(2)]

    def ln_part1(i):
        s2 = i % 2
        dma("sp", xt[s2][:], x[i * P:(i + 1) * P, :], [], ["xt%d" % s2], "xt%d" % s2)
        ln_stats(xt[s2], "xt%d" % s2, lb[s2])
        ts("dve", xn[s2][:], xt[s2][:], lb[s2]["mv"][:, 0:1], lb[s2]["rs"][:, 0:1], ALU.subtract, ALU.mult,
           ["xt%d" % s2, (lb[s2]["key"], "mv"), (lb[s2]["key"], "rs")], ["xn%d" % s2])

    def ln_part2(i):
        s2 = i % 2
        pve = pbf(6)[:, 0:512].rearrange("p (k t) -> p k t", k=4)
        pvo = pbf(7)[:, 512:1024].rearrange("p (k t) -> p k t", k=4)
        for k in range(KC):
            if k % 2 == 0:
                tr(pve[:, k // 2, :], xn[s2][:, k * P:(k + 1) * P], ident_b[:], ["xn%d" % s2, "identb"], [pbk[6]])
            else:
                tr(pvo[:, k // 2, :], xn[s2][:, k * P:(k + 1) * P], ident_b[:], ["xn%d" % s2, "identb"], [pbk[7]])
        for k in range(KC):
            dst = hT[:, k, i * P:(i + 1) * P]
            if k % 2 == 0:
                act(dst, pve[:, k // 2, :], AF.Identity, [pbk[6], "modT"], [("hT", i, k)],
                    bias=modT[:, k:k + 1], scale=modT[:, 8 + k:9 + k])
            else:
                ts("dve", dst, pvo[:, k // 2, :], modT[:, 8 + k:9 + k], modT[:, k:k + 1], ALU.mult, ALU.add,
                   [pbk[7], "modT"], [("hT", i, k)])

    def ln_tile(i):
        ln_part1(i)
        ln_part2(i)

    ucnt = {"fm": 0, "tm": 0}

    def proj_unit(g, u):
        gp = g % 2
        if u < 8:
            cb = u
            bk = ucnt["fm"] % 2
            ucnt["fm"] += 1
            for k in range(KC):
                mm(pb[bk][:], Wr[:, k, cb * P:(cb + 1) * P], hT[:, k, g * 512:(g + 1) * 512], k == 0, k == KC - 1,
                   [("Wr", cb // 4)] + [("hT", 4 * g + ti, k) for ti in range(4)], [pbk[bk]])
            if cb < 4:
                for c4 in range(4):
                    tt("dve", rqdT[gp][:, cb, c4 * P:(c4 + 1) * P], pb[bk][:, c4 * P:(c4 + 1) * P], QD[:, cb, :], ALU.mult,
                       [pbk[bk], ("QD", cb)], [("rqdT", gp, cb)])
            else:
                act(rkT[gp][:, cb - 4, :], pb[bk][:], AF.Identity, [pbk[bk]], [("rkT", gp, cb - 4)])
        else:
            ti, b3 = divmod(u - 8, 3)
            i = 4 * g + ti
            bk = 2 + (ucnt["tm"] % 2)
            ucnt["tm"] += 1
            off = 512 * (b3 + 1)
            for k in range(KC):
                mm(pb[bk][:], hT[:, k, i * P:(i + 1) * P], Wr[:, k, off:off + 512], k == 0, k == KC - 1,
                   [("Wr", b3 + 1), ("hT", i, k)], [pbk[bk]])
            if b3 == 0:
                for h in range(4):
                    act(rkd[gp][:, ti, h, :], pb[bk][:, h * P:(h + 1) * P], AF.Identity, [pbk[bk], ("KD", h)],
                        [("rkd", gp, ti, h)], scale=KD[:, h:h + 1])
            elif b3 == 1:
                cp("dve", rv[gp][:, ti, :], pb[bk][:], [pbk[bk]], [("rv", gp, ti)])
            else:
                act(rg1[:, ti, :], pb[bk][:], AF.Silu, [pbk[bk]], [("rg", ti)])

    def chunk_head(n):
        g, ti = divmod(n, 4)
        gp = g % 2
        cs = n % 2
        pS = pb[4][:].rearrange("p (h c) -> p h c", h=4)
        pY = pb[5][:].rearrange("p (h c) -> p h c", h=4)
        sl = slice(ti * P, (ti + 1) * P)
        for h in range(4):
            mm(pS[:, h, :], rkT[gp][:, h, sl], rqdT[gp][:, h, sl], True, True,
               [("rkT", gp, h), ("rqdT", gp, h)], [pbk[4]])
        tt("dve", PT[cs][:].rearrange("p h c -> p (h c)"), pb[4][:], DTp[:].rearrange("p h c -> p (h c)"), ALU.mult,
           [pbk[4]] + [("DTp", h) for h in range(4)], [("PT", cs)])
        yield
        for h in range(4):
            mm(pY[:, h, :], PT[cs][:, h, :], rv[gp][:, ti, h * P:(h + 1) * P], True, n == 0,
               [("PT", cs), ("rv", gp, ti)], [pbk[5]])
            if n > 0:
                mm(pY[:, h, :], rqdT[gp][:, h, sl], Sbf[(n - 1) % 2][:, h, :], False, True,
                   [("rqdT", gp, h), ("Sbf", (n - 1) % 2)], [pbk[5]])
        if n < NT - 1:
            pKV = pS
            for h in range(4):
                mm(pKV[:, h, :], rkd[gp][:, ti, h, :], rv[gp][:, ti, h * P:(h + 1) * P], True, True,
                   [("rkd", gp, ti, h), ("rv", gp, ti)], [pbk[4]])
            if n == 0:
                cp("dve", S32[:].rearrange("p h c -> p (h c)"), pb[4][:], [pbk[4]], ["S32"])
            else:
                for h in range(4):
                    stt(S32[:, h, :], S32[:, h, :], CD[h], pKV[:, h, :], ALU.mult, ALU.add, ["S32", pbk[4]], ["S32"])
            act(Sbf[cs][:].rearrange("p h c -> p (h c)"), S32[:].rearrange("p h c -> p (h c)"), AF.Identity, ["S32"], [("Sbf", cs)])
        for h in range(4):
            S.add("dve", lambda e, h=h, pY=pY, cs=cs: e.bn_stats(out=gst[cs][:, h, :], in_=pY[:, h, :]),
                  r=[pbk[5]], w=[("gst", cs, h)])
        for h in range(4):
            S.add("dve", lambda e, h=h, cs=cs: e.bn_aggr(out=gmv[cs][:, h, :], in_=gst[cs][:, h, :]),
                  r=[("gst", cs, h)], w=[("gmv", cs, h)])
        ts("dve", gve[cs][:], gmv[cs][:, :, 1], EPS, None, ALU.add, None, [("gmv", cs, h) for h in range(4)], [("gve", cs)])
        tt("pool", grs[cs][:], gve[cs][:], nhalf[:, 0:4], ALU.pow, [("gve", cs), "nhalf"], [("grs", cs)])
        yield
        stt(gnm[cs][:], gmv[cs][:, :, 0], -1.0, grs[cs][:], ALU.mult, ALU.mult,
            [("gmv", cs, h) for h in range(4)] + [("grs", cs)], [("gnm", cs)])
        for h in range(4):
            act(ytmp[cs][:, h, :], pY[:, h, :], AF.Identity, [pbk[5], ("grs", cs), ("gnm", cs)], [("ytmp", cs, h)],
                bias=gnm[cs][:, h:h + 1], scale=grs[cs][:, h:h + 1])
        yield

    def tail_gate(n):
        g, ti = divmod(n, 4)
        cs = n % 2
        tt("dve", ygate[cs][:], ytmp[cs][:].rearrange("p h c -> p (h c)"), rg1[:, ti, :], ALU.mult,
           [("ytmp", cs, h) for h in range(4)] + [("rg", ti)], [("ygate", cs)])

    def tail_tr(n):
        cs = n % 2
        pTr = pbf(7)[:, 0:512].rearrange("p (h c) -> p h c", h=4)
        for h in range(4):
            tr(pTr[:, h, :], ygate[cs][:, h * P:(h + 1) * P], ident_b[:], [("ygate", cs), "identb"], [pbk[7]])
        cp("dve", yTr[:, :, n * P:(n + 1) * P], pTr, [pbk[7]], [("yTr", n)])

    LNA = 9
    for i in range(4):
        ln_tile(i)
    for u in range(20):
        proj_unit(0, u)
        if u % 4 == 3:
            ln_tile(4 + u // 4)
    ln_pending = None
    for n in range(NT):
        g, ti = divmod(n, 4)
        units = [(g + 1, u) for u in (2 * ti, 2 * ti + 1, 8 + 3 * ti, 9 + 3 * ti)] if g + 1 < NG else []
        gen = chunk_head(n)
        next(gen)
        for (gg, u) in units[0:2]:
            proj_unit(gg, u)
        next(gen)
        if n > 0:
            tail_gate(n - 1)
        for (gg, u) in units[2:4]:
            proj_unit(gg, u)
        if n > 0:
            tail_tr(n - 1)
            g1_, t1_ = divmod(n - 1, 4)
            if g1_ + 1 < NG:
                proj_unit(g1_ + 1, 10 + 3 * t1_)
        if ln_pending is not None:
            ln_part2(ln_pending)
            ln_pending = None
        next(gen)
        if n + LNA < NT:
            ln_part1(n + LNA)
            ln_pending = n + LNA
        cast_ffn_piece(n)
    tail_gate(NT - 1)
    tail_tr(NT - 1)
    assert len(cast_jobs) <= NT
    S.barrier()
    A.release(m2)

    for f in range(FC):
        for hf in range(2):
            S.add("sp", lambda e, f=f, hf=hf: e.dma_start(
                out=wfi_r[f].rearrange("p (k c) -> p k c", k=KC)[:, :, hf * P:(hf + 1) * P],
                in_=wfi_sv[:, :, hf * DFF + f * P:hf * DFF + (f + 1) * P]), r=["wfis"], w=["wfir"], dma="wfir", bg=True)
    m3 = A.mark()
    Wm = A.alloc("Wm", [P, KC, 1536], BF16)
    for cbk in range(3):
        dma("pool", Wm[:, :, cbk * 512:(cbk + 1) * 512], w_in_v[:, :, 2048 + cbk * 512:2048 + (cbk + 1) * 512], [],
            [("Wm", cbk)], "Wm%d" % cbk)
    qT = A.alloc("qT", [P, T], BF16)
    kT = A.alloc("kT", [P, T], BF16)
    Va = A.alloc("Va", [P, NT, 129], BF16)
    ksum = A.alloc("ksum", [P, 16], F32)
    ksb = A.alloc("ksb", [P, 16], BF16)
    gmask = A.alloc("gmask", [P, NT, 16], F32)
    gsb = A.alloc("gsb", [P, NT, 16], F32)
    m8 = A.alloc("m8", [P, NT, 8], F32)
    thr = A.alloc("thr", [P, NT], F32)
    sel = A.alloc("sel", [P, NT, 16], F32)
    iot_i = A.alloc("ioti", [P, 32], I32)
    IOT = A.alloc("IOT", [P, 32], F32)
    AB = A.alloc("AB", [P, 32], F32)
    acc = [A.alloc("acc%d" % s, [P, 4, 129], F32) for s in range(2)]
    PTm = [A.alloc("PTm%d" % s, [P, 512], BF16) for s in range(6)]
    rinv = [A.alloc("rinv%d" % s, [P, 1], F32) for s in range(4)]
    ymo = [A.alloc("ymo%d" % s, [P, P], BF16) for s in range(4)]

    ms("dve", Va[:, :, 128:129], 1.0, ["Va1"])
    ms("pool", gmask[:], 0.0, ["gmask"])
    asel(gmask[:].rearrange("p i n -> p (i n)"), gmask[:].rearrange("p i n -> p (i n)"), [[1, 16], [0, 2], [-1, 16]],
         ALU.is_ge, NEG, -1, 0, ["gmask"], ["gmask"])
    iota(iot_i[:], [[128, 32]], -28 * 128 - 256, 1, ["ioti"])
    cp("dve", IOT[:], iot_i[:], ["ioti"], ["IOT"])

    def stage4_prologue():
        for e_ in ("act", "dve", "pool", "sp"):
            S.add(e_, lambda h: h.nop(), w=["hTall"])
        Wo = A_hT.alloc("Wo", [P, KC, D], BF16)
        for cbk in range(2):
            dma("pool", Wo[:, :, cbk * 512:(cbk + 1) * 512], w_out.rearrange("(k p) n -> p k n", p=P)[:, :, cbk * 512:(cbk + 1) * 512],
                [], [("Wo", cbk)], "Wo%d" % cbk)
        lnT = A_hT.alloc("lnT", [P, 4, D], F32)
        g2bc = A_hT.alloc("g2bc", [P, D], F32)
        tmp2 = [A_hT.alloc("tmp2_%d" % s, [P, 512], F32) for s in range(2)]
        actT = A_hT.alloc("actT", [P, FC, 512], BF16)
        A_pro = Arena(nc, A_hT.top - FC * 512 * 2, A_hT.top)
        lnrow = A_pro.alloc("lnrow", [1, 1, D], F32)
        g1bc = A_pro.alloc("g1bc", [P, D], F32)
        Gt = [A_pro.alloc("Gt%d" % s, [P, P], F32) for s in range(2)]
        bc = 0
        for q, src in enumerate((ln1_w, ln1_b, ln2_w, ln2_b)):
            dma("sp", lnrow[0:1, 0, :], src, [], ["lnrow"], "lnrow")
            for cbk in range(2):
                bk = bc % 4
                bc += 1
                mm(pb[bk][:], ones_f[0:1, 0:P], lnrow[0:1, 0, cbk * 512:(cbk + 1) * 512], True, True, ["lnrow", "onesf"], [pbk[bk]])
                cp("dve", lnT[:, q, cbk * 512:(cbk + 1) * 512], pb[bk][:], [pbk[bk]], [("lnT", q)])
        for q, (base, dstt) in enumerate(((16, g1bc), (40, g2bc))):
            for cbk in range(2):
                bk = bc % 4
                bc += 1
                for jj in range(4):
                    j = cbk * 4 + jj
                    gs = (q * 8 + j) % 2
                    ts("dve", Gt[gs][:], ones_f[:], modT[:, base + j:base + j + 1], None, ALU.mult, None, ["onesf", "modT"], [("Gt", gs)])
                    mm(pb[bk][:, jj * P:(jj + 1) * P], Gt[gs][:], ident_f[:], True, True, [("Gt", gs), "identf"], [pbk[bk]])
                cp("dve", dstt[:, cbk * 512:(cbk + 1) * 512], pb[bk][:], [pbk[bk]], [("gbc", q)])
        for k in range(KC):
            tt("dve", Wo[:, k, :], Wo[:, k, :], g1bc[:], ALU.mult, [("Wo", 0), ("Wo", 1), ("gbc", 0)], [("Wof", k)])
        return Wo, lnT, g2bc, tmp2, actT

    mst = {"ocnt": 0, "ptc": 0, "trc": 0}
    NPT = 6
    for hm in range(4):
        hkey = lambda g: [("hT", 4 * g + ti, k) for ti in range(4) for k in range(KC)]
        for g in range(NG):
            bk = g % 2
            for k in range(KC):
                mm(pb[bk][:], Wm[:, k, 512 + hm * P:512 + (hm + 1) * P], hT[:, k, g * 512:(g + 1) * 512], k == 0, k == KC - 1,
                   [("Wm", 1), "hTall"], [pbk[bk]])
            act(kT[:, g * 512:(g + 1) * 512], pb[bk][:], AF.Identity, [pbk[bk]], [("kT", g), ("bser", bk)])
            S.add("dve", lambda e, bk=bk, g=g: e.tensor_reduce(out=ksum[:, 2 * g:2 * g + 2],
                                                               in_=pb[bk][:].rearrange("p (b s) -> p b s", b=2),
                                                               axis=AX.X, op=ALU.add),
                  r=[pbk[bk]], w=[("ksum", g), ("bser", bk)])
        cp("dve", ksb[:], ksum[:], [("ksum", g) for g in range(NG)], ["ksb"])
        for g in range(NG):
            bk = g % 2
            for k in range(KC):
                mm(pb[bk][:], Wm[:, k, hm * P:(hm + 1) * P], hT[:, k, g * 512:(g + 1) * 512], k == 0, k == KC - 1,
                   [("Wm", 0), "hTall"], [pbk[bk]])
            act(qT[:, g * 512:(g + 1) * 512], pb[bk][:], AF.Identity, [pbk[bk]], [("qT", g)])
        pg = pb[2][:].rearrange("p (i n) -> p i n", n=16)
        for i in range(NT):
            mm(pg[:, i, :], qT[:, i * P:(i + 1) * P], ksb[:], True, True, [("qT", i // 4), "ksb"], [pbk[2]])
        tt("dve", gsb[:].rearrange("p i n -> p (i n)"), pb[2][:], gmask[:].rearrange("p i n -> p (i n)"), ALU.add,
           [pbk[2], "gmask"], ["gsb"])
        for i in range(2, NT):
            S.add("dve", lambda e, i=i: e.max(out=m8[:, i, :], in_=gsb[:, i, :]), r=["gsb"], w=[("m8", i)])
        ts("dve", thr[:, 2:NT], m8[:, 2:NT, 2], -1.0e29, None, ALU.max, None, [("m8", i) for i in range(2, NT)], ["thr"])
        for i in range(2, NT):
            ts("dve", sel[:, i, :], gsb[:, i, :], thr[:, i:i + 1], None, ALU.is_ge, None, ["gsb", "thr"], [("sel", i)])
        for i in range(NT):
            bk = 6 + (i % 2)
            for k in range(KC):
                mm(pb[bk][:, 0:P], hT[:, k, i * P:(i + 1) * P], Wm[:, k, 1024 + hm * P:1024 + (hm + 1) * P], k == 0, k == KC - 1,
                   [("Wm", 2), "hTall"], [pbk[bk]])
            act(Va[:, i, 0:P], pb[bk][:, 0:P], AF.Identity, [pbk[bk]], [("Va", i)])
        ts("dve", AB[:], IOT[:], SLOPES[hm], None, ALU.mult, None, ["IOT"], ["AB"])
        def phase1(gq, n, st):
            qts = [ti for ti in range(4) if (2 * gq + ti // 2) >= n]
            pts = {}
            for kt in (2 * n, 2 * n + 1):
                qk = [ti for ti in qts if 4 * gq + ti >= kt]
                if not qk:
                    continue
                c0 = min(qk) * P
                sb = (0, 1, 6, 7)[st["ptc"] % 4]
                ptb = PTm[st["ptc"] % NPT]
                pkey = ("PTm", st["ptc"] % NPT)
                st["ptc"] += 1
                pts[kt] = (ptb, pkey, qk)
                mm(pb[sb][:, c0:512], kT[:, kt * P:(kt + 1) * P], qT[:, gq * 512 + c0:(gq + 1) * 512], True, True,
                   [("kT", kt // 4), ("qT", gq)], [pbk[sb]])
                rel = kt - 4 * gq + 28
                act(ptb[:, c0:512], pb[sb][:, c0:512], AF.Exp, [pbk[sb], "AB"], [pkey], bias=AB[:, rel:rel + 1], scale=SCALE)
                if kt >= 4 * gq:
                    td = kt - 4 * gq
                    tt("dve", ptb[:, td * P:(td + 1) * P], ptb[:, td * P:(td + 1) * P], cmask_b[:], ALU.mult,
                       [pkey, "cmaskb"], [pkey])
            return qts, pts

        def phase2(gq, n, qts, pts, st):
            accs = acc[gq % 2]
            ak = gq % 2
            oset = st["ocnt"] % 2
            st["ocnt"] += 1
            banks = (2 + 2 * oset, 3 + 2 * oset)
            for ti in qts:
                bank = banks[ti // 2]
                reg = pb[bank][:, (ti % 2) * 129:(ti % 2) * 129 + 129]
                kts = [kt for kt in (2 * n, 2 * n + 1) if kt in pts and ti in pts[kt][2]]
                for kt in kts:
                    ptb, pkey, _ = pts[kt]
                    mm(reg, ptb[:, ti * P:(ti + 1) * P], Va[:, kt, :], kt == kts[0], kt == kts[-1],
                       [pkey, ("Va", kt), "Va1"], [pbk[bank]])
            for ti in qts:
                qi = 4 * gq + ti
                bank = banks[ti // 2]
                reg = pb[bank][:, (ti % 2) * 129:(ti % 2) * 129 + 129]
                own = (n == qi // 2)
                sc = 1.0 if own else sel[:, qi, n:n + 1]
                rds = [pbk[bank]] + ([] if own else [("sel", qi)])
                if n == 0:
                    ts("dve", accs[:, ti, :], reg, sc, None, ALU.mult, None, rds, [("acc", ak, ti)])
                else:
                    stt(accs[:, ti, :], reg, sc, accs[:, ti, :], ALU.mult, ALU.add, rds + [("acc", ak, ti)], [("acc", ak, ti)])

        def fin_dve(gq):
            accs = acc[gq % 2]
            ak = gq % 2
            for ti in range(4):
                S.add("dve", lambda e, ti=ti, accs=accs: e.reciprocal(out=rinv[ti][:], in_=accs[:, ti, 128:129]),
                      r=[("acc", ak, ti)], w=[("rinv", ti)])
                ts("dve", ymo[ti][:], accs[:, ti, 0:P], rinv[ti][:, 0:1], None, ALU.mult, None,
                   [("acc", ak, ti), ("rinv", ti)], [("ymo", ti)])

        def fin_pe(gq):
            tb = (0, 1, 6, 7)[mst["ptc"] % 4]
            mst["ptc"] += 1
            for ti in range(4):
                qi = 4 * gq + ti
                pT_ = pbf(tb)[:, ti * P:(ti + 1) * P]
                tr(pT_, ymo[ti][:], ident_b[:], [("ymo", ti), "identb"], [pbk[tb]])
            for ti in range(4):
                qi = 4 * gq + ti
                pT_ = pbf(tb)[:, ti * P:(ti + 1) * P]
                cp("dve", yTm[:, hm, qi * P:(qi + 1) * P], pT_, [pbk[tb]], [("yTm", hm, qi)])

        blocks = [(gq, n) for gq in range(NG) for n in range(2 * gq + 2)]
        pend = None
        fin_pending = None
        for (gq, n) in blocks:
            qts, pts = phase1(gq, n, mst)
            if pend is not None:
                phase2(*pend, mst)
                if fin_pending is not None:
                    fin_pe(fin_pending)
                    fin_pending = None
                if pend[1] == 2 * pend[0] + 1:
                    fin_dve(pend[0])
                    fin_pending = pend[0]
            pend = (gq, n, qts, pts)
        phase2(*pend, mst)
        if fin_pending is not None:
            fin_pe(fin_pending)
        fin_dve(pend[0])
        fin_pe(pend[0])
    S.barrier()
    A.release(m3)

    Wo, lnT, g2bc, tmp2, actT = stage4_prologue()
    S.barrier()
    x1 = [[A.alloc("x1_%d_%d" % (gp, s), [P, D], F32) for s in range(4)] for gp in range(2)]
    xn2 = [A.alloc("xn2_%d" % s, [P, D], BF16) for s in range(4)]
    h2T = A.alloc("h2T", [P, KC, 512], BF16)
    sg = [A.alloc("sg%d" % s, [P, 512], F32) for s in range(2)]
    NWI = 3
    NWO = 4
    wfi = [A.alloc("wfi%d" % s, [P, KC, 256], BF16) for s in range(3)]
    wfo = [A.alloc("wfo%d" % s, [P, D], BF16) for s in range(NWO)]
    lf = [ln_bufs("lf%d" % s) for s in range(4)]
    lc = [ln_bufs("lc%d" % s) for s in range(4)]

    def yT_sl(hh, i):
        if hh < 4:
            return yTr[:, hh, i * P:(i + 1) * P]
        return yTm[:, hh - 4, i * P:(i + 1) * P]

    def ln_finish(buf, key, lb):
        kp = lb["key"]
        stt(lb["nm"][:], lb["mv"][:, 0:1], -1.0, lb["rs"][:], ALU.mult, ALU.mult, [(kp, "mv"), (kp, "rs")], [(kp, "nm")])

    def ln_norm(buf, key, lb):
        kp = lb["key"]
        act(buf[:], buf[:], AF.Identity, [key, (kp, "rs"), (kp, "nm")], [key], bias=lb["nm"][:, 0:1], scale=lb["rs"][:, 0:1])

    def ln_affine(buf, key, wq):
        tt("dve", buf[:], buf[:], lnT[:, wq, :], ALU.mult, [key, ("lnT", wq)], [key])
        tt("dve", buf[:], buf[:], lnT[:, wq + 1, :], ALU.add, [key, ("lnT", wq + 1)], [key])

    def front_a(g):
        gp = g % 2
        for ti in range(4):
            i = 4 * g + ti
            dma("sp", x1[gp][ti][:], x[i * P:(i + 1) * P, :], [], [("x1", gp, ti)], "xld%d_%d" % (gp, ti))
        for ti in range(4):
            i = 4 * g + ti
            banks = (2 * (ti % 2), 2 * (ti % 2) + 1)
            for cbk in range(2):
                for hh in range(8):
                    mm(pb[banks[cbk]][:], yT_sl(hh, i), Wo[:, hh, cbk * 512:(cbk + 1) * 512], hh == 0, hh == 7,
                       [("Wof", hh)], [pbk[banks[cbk]]])
            for cbk in range(2):
                hs = slice(cbk * 512, (cbk + 1) * 512)
                stt(x1[gp][ti][:, hs], x1[gp][ti][:, hs], ALPHA, pb[banks[cbk]][:], ALU.mult, ALU.add,
                    [("x1", gp, ti), pbk[banks[cbk]]], [("x1", gp, ti)])
            ln_stats(x1[gp][ti], ("x1", gp, ti), lf[ti])
        for ti in range(4):
            ln_finish(x1[gp][ti], ("x1", gp, ti), lf[ti])
        for ti in range(4):
            ln_norm(x1[gp][ti], ("x1", gp, ti), lf[ti])
        for ti in range(4):
            ln_affine(x1[gp][ti], ("x1", gp, ti), 0)
            ln_stats(x1[gp][ti], ("x1", gp, ti), lf[ti])
        for ti in range(4):
            ts("dve", xn2[ti][:], x1[gp][ti][:], lf[ti]["mv"][:, 0:1], lf[ti]["rs"][:, 0:1], ALU.subtract, ALU.mult,
               [("x1", gp, ti), (lf[ti]["key"], "mv"), (lf[ti]["key"], "rs")], [("xn2", ti)])

    def front_b(g):
        for ti in range(4):
            bke, bko = 4 + 2 * (ti % 2), 5 + 2 * (ti % 2)
            pve = pbf(bke).rearrange("p (k t) -> p k t", k=KC)
            pvo = pbf(bko).rearrange("p (k t) -> p k t", k=KC)
            for k in range(KC):
                if k % 2 == 0:
                    tr(pve[:, k // 2, :], xn2[ti][:, k * P:(k + 1) * P], ident_b[:], [("xn2", ti), "identb"], [pbk[bke]])
                else:
                    tr(pvo[:, k // 2, :], xn2[ti][:, k * P:(k + 1) * P], ident_b[:], [("xn2", ti), "identb"], [pbk[bko]])
            for k in range(KC):
                dst = h2T[:, k, ti * P:(ti + 1) * P]
                if k % 2 == 0:
                    act(dst, pve[:, k // 2, :], AF.Identity, [pbk[bke], "modT"], [("h2T", ti, k)],
                        bias=modT[:, 24 + k:25 + k], scale=modT[:, 32 + k:33 + k])
                else:
                    ts("dve", dst, pvo[:, k // 2, :], modT[:, 32 + k:33 + k], modT[:, 24 + k:25 + k], ALU.mult, ALU.add,
                       [pbk[bko], "modT"], [("h2T", ti, k)])

    cnt = {"wi": 0, "wo": 0}

    def ffn_in(g, pending):
        for f in range(FC):
            if pending:
                pending.pop(0)()
            sl_ = cnt["wi"] % NWI
            cnt["wi"] += 1
            dma("sp", wfi[sl_][:].rearrange("p k c -> p (k c)"), wfi_r[f], ["wfir"], [("wfi", sl_)], "wfi%d" % sl_)
            gb, ub = 4 + 2 * (f % 2), 5 + 2 * (f % 2)
            for half, bk in ((0, gb), (1, ub)):
                for k in range(KC):
                    mm(pb[bk][:], wfi[sl_][:, k, half * P:(half + 1) * P], h2T[:, k, :], k == 0, k == KC - 1,
                       [("wfi", sl_)] + [("h2T", ti, k) for ti in range(4)], [pbk[bk]])
            act(sg[f % 2][:], pb[gb][:], AF.Silu, [pbk[gb]], [("sg", f % 2)])
            tt("dve", actT[:, f, :], sg[f % 2][:], pb[ub][:], ALU.mult, [("sg", f % 2), pbk[ub]], [("actT", f)])

    def ffn_out(g):
        for f in range(FC):
            sl_ = cnt["wo"] % NWO
            cnt["wo"] += 1
            dma("sp", wfo[sl_][:], wfo_s[f * P:(f + 1) * P, :], ["wfos"], [("wfo", sl_)], "wfo%d" % sl_)
            for ti in range(4):
                for cbk in range(2):
                    bk = ti * 2 + cbk
                    mm(pb[bk][:], actT[:, f, ti * P:(ti + 1) * P], wfo[sl_][:, cbk * 512:(cbk + 1) * 512], f == 0, f == FC - 1,
                       [("actT", f), ("wfo", sl_)], [pbk[bk]])

    def tail_a1(g):
        gp = g % 2
        tc = 0
        for ti in (2, 3, 0, 1):
            for cbk in range(2):
                hs = slice(cbk * 512, (cbk + 1) * 512)
                bk = ti * 2 + cbk
                t2 = tmp2[tc % 2]
                tk = ("tmp2", tc % 2)
                tc += 1
                tt("dve", t2[:], pb[bk][:], g2bc[:, hs], ALU.mult, [pbk[bk], ("gbc", 1)], [tk])
                stt(x1[gp][ti][:, hs], x1[gp][ti][:, hs], ALPHA, t2[:], ALU.mult, ALU.add, [("x1", gp, ti), tk], [("x1", gp, ti)])

    def tail_items(g):
        gp = g % 2
        items = []
        for ti in range(4):
            items.append(lambda ti=ti: ln_stats(x1[gp][ti], ("x1", gp, ti), lc[ti]))
        for ti in range(4):
            items.append(lambda ti=ti: ln_finish(x1[gp][ti], ("x1", gp, ti), lc[ti]))
        for ti in range(4):
            items.append(lambda ti=ti: ln_norm(x1[gp][ti], ("x1", gp, ti), lc[ti]))
        for ti in range(4):
            items.append(lambda ti=ti: tt("dve", x1[gp][ti][:], x1[gp][ti][:], lnT[:, 2, :], ALU.mult,
                                          [("x1", gp, ti), ("lnT", 2)], [("x1", gp, ti)]))
        for ti in range(4):
            def fin(ti=ti):
                tt("dve", x1[gp][ti][:], x1[gp][ti][:], lnT[:, 3, :], ALU.add, [("x1", gp, ti), ("lnT", 3)], [("x1", gp, ti)])
                i = 4 * g + ti
                S.wait_at_end(dma("pool", y[i * P:(i + 1) * P, :], x1[gp][ti][:], [("x1", gp, ti)], [("y", i)], "yout%d_%d" % (gp, ti)))
            items.append(fin)
        return items

    front_a(0)
    front_b(0)
    pending = []
    for g in range(NG):
        ffn_in(g, pending)
        assert not pending
        if g + 1 < NG:
            front_a(g + 1)
        ffn_out(g)
        tail_a1(g)
        if g + 1 < NG:
            front_b(g + 1)
        pending = tail_items(g)
    for it in pending:
        it()
    S.emit()
    return nc


_NC_CACHE = {}


def kernel(x, c, w_ada, b_ada, w_in, w_out, ln1_w, ln1_b, w_ffn_in, w_ffn_out, ln2_w, ln2_b):
    f = lambda a: np.ascontiguousarray(np.asarray(a, dtype=np.float32))
    x = f(x)
    c = f(c)
    B = x.shape[0]
    if "nc" not in _NC_CACHE:
        _NC_CACHE["nc"] = build_nc()
    nc = _NC_CACHE["nc"]
    shared = {
        "w_ada": f(w_ada)[0], "b_ada": f(b_ada)[0:1], "w_in": f(w_in)[0], "w_out": f(w_out)[0],
        "ln1_w": f(ln1_w)[0:1], "ln1_b": f(ln1_b)[0:1], "w_ffn_in": f(w_ffn_in)[0], "w_ffn_out": f(w_ffn_out)[0],
        "ln2_w": f(ln2_w)[0:1], "ln2_b": f(ln2_b)[0:1],
    }
    in_maps = []
    for b in range(B):
        m = dict(shared)
        m["x"] = x[b]
        m["c"] = c[b:b + 1]
        in_maps.append(m)
    res = run_bass_kernel_spmd(nc, in_maps, core_ids=list(range(B)))
    return np.stack([np.asarray(r["y"], dtype=np.float32) for r in res.results], axis=0)
```

```python
import contextlib
import math
import numpy as np
import concourse.bass as bass
import concourse.mybir as mybir
from concourse.bass_utils import run_bass_kernel_spmd

F32 = mybir.dt.float32
BF16 = mybir.dt.bfloat16
I32 = mybir.dt.int32
AF = mybir.ActivationFunctionType
ALU = mybir.AluOpType
AX = mybir.AxisListType

T = 4096
D = 1024
P = 128
KC = 8
NT = 32
NG = 8
DFF = 2816
FC = 22
ALPHA = 2.0 ** 0.25
EPS = 1e-5
SCALE = 128.0 ** -0.5
LG = [math.log(1.0 - 2.0 ** (-5.0 - h)) for h in range(4)]
SLOPES = [2.0 ** (-8.0 * (h + 1.0) / 4.0) for h in range(4)]
NEG = -1.0e30

COMPUTE = ("pe", "act", "dve", "pool")
ALL_ENG = ("pe", "act", "dve", "pool", "sp")


class _Op:
    __slots__ = ("eng", "fn", "deps", "sig", "sig_idx", "dma", "dcount", "pos", "bg")

    def __init__(self, eng, fn, dma):
        self.bg = False
        self.eng = eng
        self.fn = fn
        self.deps = []
        self.sig = False
        self.sig_idx = 0
        self.dma = dma
        self.dcount = 0
        self.pos = 0


class Sched:
    def __init__(self, nc):
        self.nc = nc
        self.ops = {e: [] for e in ALL_ENG}
        self.last_w = {}
        self.readers = {}
        self.dma_streams = {}
        self.final_waits = []
        self.dma_since_barrier = []

    def add(self, eng, fn, r=(), w=(), dma=None, extra_deps=(), bg=False):
        op = _Op(eng, fn, dma)
        op.bg = bg
        op.pos = len(self.ops[eng])
        deps = {}

        def add_dep(d, war):
            if d is op:
                return
            if d.dma is None and d.eng == eng:
                if eng == "pe" or war:
                    return
            key = id(d) if d.dma is not None else d.eng
            old = deps.get(key)
            if old is None or (d.dma is None and d.pos > old.pos):
                deps[key] = d

        for k in r:
            lw = self.last_w.get(k)
            if lw is not None:
                add_dep(lw, False)
        for k in w:
            lw = self.last_w.get(k)
            if lw is not None:
                add_dep(lw, False)
            rd = self.readers.get(k)
            if rd:
                for d in rd.values():
                    add_dep(d, True)
        for d in extra_deps:
            key = id(d) if d.dma is not None else d.eng
            old = deps.get(key)
            if old is None or (d.dma is None and d.pos > old.pos):
                if not (d.dma is None and d.eng == eng):
                    deps[key] = d
        op.deps = list(deps.values())
        for k in w:
            self.last_w[k] = op
            self.readers[k] = {}
        for k in r:
            rd = self.readers.setdefault(k, {})
            rd[id(op) if dma is not None else eng] = op
        if dma is not None:
            n = self.dma_streams.get(dma, 0) + 1
            self.dma_streams[dma] = n
            op.dcount = 16 * n
            if not bg:
                self.dma_since_barrier.append(op)
        self.ops[eng].append(op)
        return op

    def barrier(self):
        lasts = [self.ops[e][-1] for e in COMPUTE if self.ops[e]]
        dmas = list(self.dma_since_barrier)
        per = {}
        for d in dmas:
            per[d.dma] = d
        deps = lasts + list(per.values())
        for e in ALL_ENG:
            self.add(e, lambda h: h.nop(), extra_deps=deps)
        self.last_w = {k: v for k, v in self.last_w.items() if v.bg}
        self.readers = {}
        self.dma_since_barrier = []

    def wait_at_end(self, op):
        self.final_waits.append(op)

    def emit(self):
        nc = self.nc
        for e in ALL_ENG:
            for op in self.ops[e]:
                for d in op.deps:
                    if d.dma is None:
                        d.sig = True
        for e in ALL_ENG:
            n = 0
            for op in self.ops[e]:
                if op.sig:
                    n += 1
                    op.sig_idx = n
        with contextlib.ExitStack() as es:
            esem = {e: es.enter_context(nc.semaphore("s_" + e)) for e in ALL_ENG}
            dsem = {name: es.enter_context(nc.semaphore("d_%d" % i))
                    for i, name in enumerate(self.dma_streams)}
            block = es.enter_context(nc.Block())

            def run(e, handle):
                seen = {}
                for op in self.ops[e]:
                    waits = {}
                    for d in op.deps:
                        if d.dma is not None:
                            s, v = dsem[d.dma], d.dcount
                        else:
                            s, v = esem[d.eng], d.sig_idx
                        key = id(s)
                        if key not in waits or waits[key][1] < v:
                            waits[key] = (s, v)
                    for key, (s, v) in waits.items():
                        if seen.get(key, 0) >= v:
                            continue
                        seen[key] = v
                        handle.wait_ge(s, v)
                    ins = op.fn(handle)
                    if op.dma is not None:
                        ins.then_inc(dsem[op.dma], 16)
                    elif op.sig:
                        ins.then_inc(esem[e], 1)
                if e == "sp":
                    for op in self.final_waits:
                        handle.wait_ge(dsem[op.dma], op.dcount)

            @block.tensor
            def _(h):
                run("pe", h)

            @block.scalar
            def _(h):
                run("act", h)

            @block.vector
            def _(h):
                run("dve", h)

            @block.gpsimd
            def _(h):
                run("pool", h)

            @block.sync
            def _(h):
                run("sp", h)


class Arena:
    _n = [0]

    def __init__(self, nc, lo, hi):
        self.nc = nc
        self.LO = lo
        self.HI = hi
        self.top = lo
        self.peak = 0

    def alloc(self, name, shape, dt):
        esz = {F32: 4, BF16: 2, I32: 4}[dt]
        size = esz
        for s in shape[1:]:
            size *= s
        size = (size + 63) // 64 * 64
        Arena._n[0] += 1
        t = self.nc.alloc_sbuf_tensor_at("%s_%d" % (name, Arena._n[0]), list(shape), dt, offset=self.top)
        self.top += size
        self.peak = max(self.peak, self.top)
        assert self.top <= self.HI, "SBUF overflow at %s: %d" % (name, self.top)
        return t

    def mark(self):
        return self.top

    def release(self, m):
        self.top = m


def build_nc():
    nc = bass.Bass("TRN2", target_bir_lowering=False)
    x = nc.dram_tensor("x", [T, D], F32, kind="ExternalInput").ap()
    c = nc.dram_tensor("c", [1, D], F32, kind="ExternalInput").ap()
    w_ada = nc.dram_tensor("w_ada", [D, 6 * D], F32, kind="ExternalInput").ap()
    b_ada = nc.dram_tensor("b_ada", [1, 6 * D], F32, kind="ExternalInput").ap()
    w_in = nc.dram_tensor("w_in", [D, 3584], F32, kind="ExternalInput").ap()
    w_out = nc.dram_tensor("w_out", [D, D], F32, kind="ExternalInput").ap()
    ln1_w = nc.dram_tensor("ln1_w", [1, D], F32, kind="ExternalInput").ap()
    ln1_b = nc.dram_tensor("ln1_b", [1, D], F32, kind="ExternalInput").ap()
    w_ffn_in = nc.dram_tensor("w_ffn_in", [D, 2 * DFF], F32, kind="ExternalInput").ap()
    w_ffn_out = nc.dram_tensor("w_ffn_out", [DFF, D], F32, kind="ExternalInput").ap()
    ln2_w = nc.dram_tensor("ln2_w", [1, D], F32, kind="ExternalInput").ap()
    ln2_b = nc.dram_tensor("ln2_b", [1, D], F32, kind="ExternalInput").ap()
    y = nc.dram_tensor("y", [T, D], F32, kind="ExternalOutput").ap()
    wfi_s = nc.dram_tensor("wfi_s", [D, 2 * DFF], BF16, kind="Internal").ap()
    wfo_s = nc.dram_tensor("wfo_s", [DFF, D], BF16, kind="Internal").ap()
    wfi_r = nc.dram_tensor("wfi_r", [FC, P, KC * 256], BF16, kind="Internal").ap()

    S = Sched(nc)
    SB_LO, SB_HI = 16512, 229376
    o_c = SB_LO
    o_yTr = o_c + 4096
    o_yTm = o_yTr + 32768
    o_hT = o_yTm + 32768
    o_tail = o_hT + 65536
    AC = Arena(nc, o_c, o_yTr)
    A = Arena(nc, o_tail, SB_HI)
    A_yTm = Arena(nc, o_yTm, o_hT)
    A_hT = Arena(nc, o_hT, o_tail)
    pb = [nc.alloc_psum_tensor("pb%d" % i, [P, 512], F32) for i in range(8)]
    pbk = ["pb%d" % i for i in range(8)]

    def pbf(i):
        return pb[i][:].bitcast(BF16)

    def mm(out, lhsT, rhs, start, stop, r, w):
        return S.add("pe", lambda e: e.matmul(out, lhsT=lhsT, rhs=rhs, start=start, stop=stop), r=r, w=w)

    def tr(out, in_, ident, r, w):
        return S.add("pe", lambda e: e.transpose(out=out, in_=in_, identity=ident), r=r, w=w)

    def act(out, in_, func, r, w, bias=None, scale=None):
        kw = {}
        if bias is not None:
            kw["bias"] = bias
        if scale is not None:
            kw["scale"] = scale
        return S.add("act", lambda e: e.activation(out=out, in_=in_, func=func, **kw), r=r, w=w)

    def ts(eng, out, in0, s1, s2, op0, op1, r, w):
        if op1 is None:
            return S.add(eng, lambda e: e.tensor_scalar(out=out, in0=in0, scalar1=s1, scalar2=None, op0=op0), r=r, w=w)
        return S.add(eng, lambda e: e.tensor_scalar(out=out, in0=in0, scalar1=s1, scalar2=s2, op0=op0, op1=op1), r=r, w=w)

    def tt(eng, out, in0, in1, op, r, w):
        return S.add(eng, lambda e: e.tensor_tensor(out=out, in0=in0, in1=in1, op=op), r=r, w=w)

    def stt(out, in0, scalar, in1, op0, op1, r, w):
        return S.add("dve", lambda e: e.scalar_tensor_tensor(out=out, in0=in0, scalar=scalar, in1=in1, op0=op0, op1=op1), r=r, w=w)

    def cp(eng, out, in_, r, w):
        return S.add(eng, lambda e: e.tensor_copy(out=out, in_=in_), r=r, w=w)

    def ms(eng, ap, val, w):
        return S.add(eng, lambda e: e.memset(ap, val), w=w)

    def dma(eng, out, in_, r, w, stream):
        return S.add(eng, lambda e: e.dma_start(out=out, in_=in_), r=r, w=w, dma=stream)

    def iota(out, pattern, base, cm, w):
        return S.add("pool", lambda e: e.iota(out, pattern=pattern, base=base, channel_multiplier=cm), w=w)

    def asel(out, in_, pattern, op, fill, base, cm, r, w):
        return S.add("pool", lambda e: e.affine_select(out=out, in_=in_, pattern=pattern, compare_op=op, fill=fill,
                                                       base=base, channel_multiplier=cm), r=r, w=w)

    ident_f = AC.alloc("identf", [P, P], F32)
    ident_b = AC.alloc("identb", [P, P], BF16)
    ones_f = AC.alloc("onesf", [P, P], F32)
    cmask_f = AC.alloc("cmaskf", [P, P], F32)
    cmask_b = AC.alloc("cmaskb", [P, P], BF16)
    nmask_b = AC.alloc("nmaskb", [P, P], BF16)
    nhalf = AC.alloc("nhalf", [P, 8], F32)
    modT = AC.alloc("modT", [P, 48], F32)
    lnsc = AC.alloc("lnsc", [P, 1], F32)
    ms("pool", ones_f[:], 1.0, ["onesf"])
    ms("pool", ident_f[:], 1.0, ["identf"])
    asel(ident_f[:], ident_f[:], [[-1, P]], ALU.is_equal, 0.0, 0, 1, ["identf"], ["identf"])
    cp("dve", ident_b[:], ident_f[:], ["identf"], ["identb"])
    ms("pool", cmask_f[:], 1.0, ["cmaskf"])
    asel(cmask_f[:], cmask_f[:], [[1, P]], ALU.is_ge, 0.0, 0, -1, ["cmaskf"], ["cmaskf"])
    cp("dve", cmask_b[:], cmask_f[:], ["cmaskf"], ["cmaskb"])
    ts("dve", nmask_b[:], cmask_f[:], -1.0, 30000.0, ALU.add, ALU.mult, ["cmaskf"], ["nmaskb"])
    ms("pool", nhalf[:], -0.5, ["nhalf"])
    ms("pool", lnsc[:], math.log(SCALE), ["lnsc"])
    one11 = ones_f[0:1, 0:1]

    hT = nc.alloc_sbuf_tensor_at("hT", [P, KC, T], BF16, offset=o_hT)
    yTr = nc.alloc_sbuf_tensor_at("yTr", [P, 4, T], BF16, offset=o_yTr)
    yTm = nc.alloc_sbuf_tensor_at("yTm", [P, 4, T], BF16, offset=o_yTm)

    cast_jobs = []
    for k in range(KC):
        for hf in range(2):
            cast_jobs.append((wfi_s[k * P:(k + 1) * P, hf * DFF:(hf + 1) * DFF], w_ffn_in[k * P:(k + 1) * P, hf * DFF:(hf + 1) * DFF], "wfis"))
    for f0 in range(0, FC, 2):
        cast_jobs.append((wfo_s[f0 * P:(f0 + 2) * P, :], w_ffn_out[f0 * P:(f0 + 2) * P, :], "wfos"))

    def cast_ffn_piece(j):
        if j < len(cast_jobs):
            o_, i_, key_ = cast_jobs[j]
            S.add("pool", lambda e: e.dma_start(out=o_, in_=i_), w=[key_], dma=key_, bg=True)
    wfi_sv = wfi_s.rearrange("(k p) n -> p k n", p=P)

    w_in_v = w_in.rearrange("(k p) n -> p k n", p=P)
    Wr = A_yTm.alloc("Wr", [P, KC, 2048], BF16)
    for cbk in range(4):
        dma("pool", Wr[:, :, cbk * 512:(cbk + 1) * 512], w_in_v[:, :, cbk * 512:(cbk + 1) * 512], [], [("Wr", cbk)], "Wr%d" % cbk)

    m0 = A.mark()
    NB0 = 24
    CW = 256
    c_row = A.alloc("crow", [1, D], F32)
    bada = [A.alloc("bada%d" % s, [1, CW], F32) for s in range(2)]
    modrow = A.alloc("modrow", [1, 6 * D], F32)
    scT = A.alloc("scT", [P, KC], F32)
    screp = A.alloc("screp", [P, KC, P], BF16)
    NWA = 4
    wa = [A.alloc("wa%d" % s, [P, KC, CW], F32) for s in range(NWA)]
    wab = [A.alloc("wab%d" % s, [P, KC, CW], BF16) for s in range(2)]
    dma("sp", c_row[:], c, [], ["crow"], "crow")
    for k in range(KC):
        mm(pb[0][:, k:k + 1], c_row[0:1, k * P:(k + 1) * P], one11, True, True, ["crow", "onesf"], [pbk[0]])
    act(scT[:], pb[0][:, 0:KC], AF.Silu, [pbk[0]], ["scT"])
    for k in range(KC):
        ts("dve", screp[:, k, :], ones_f[:], scT[:, k:k + 1], None, ALU.mult, None, ["onesf", "scT"], [("screp", k)])
    wada_v = w_ada.rearrange("(k p) n -> p k n", p=P)
    for blk in range(NB0):
        s = blk % 2
        s4_ = blk % NWA
        bk = 2 + (blk % 2)
        dma("sp", bada[s][:], b_ada[0:1, blk * CW:(blk + 1) * CW], [], ["bada%d" % s], "bada%d" % s)
        dma("sp", wa[s4_][:], wada_v[:, :, blk * CW:(blk + 1) * CW], [], ["wa%d" % s4_], "wa%d" % s4_)
        if blk % 2 == 0:
            cp("dve", wab[s][:].rearrange("p k n -> p (k n)"), wa[s4_][:].rearrange("p k n -> p (k n)"), ["wa%d" % s4_], ["wab%d" % s])
        else:
            act(wab[s][:].rearrange("p k n -> p (k n)"), wa[s4_][:].rearrange("p k n -> p (k n)"), AF.Identity, ["wa%d" % s4_], ["wab%d" % s])
        for k in range(KC):
            mm(pb[bk][:, 0:CW], screp[:, k, :], wab[s][:, k, :], k == 0, k == KC - 1, ["wab%d" % s, ("screp", k)], [pbk[bk]])
        tt("dve", modrow[0:1, blk * CW:(blk + 1) * CW], pb[bk][0:1, 0:CW], bada[s][0:1, :], ALU.add,
           [pbk[bk], "bada%d" % s], ["modrow"])
    for j in range(48):
        mm(pb[1][:, j:j + 1], modrow[0:1, j * P:(j + 1) * P], one11, True, True, ["modrow", "onesf"], [pbk[1]])
    cp("dve", modT[:], pb[1][:, 0:48], [pbk[1]], ["modT"])
    ts("dve", modT[:, 8:16], modT[:, 8:16], 1.0, None, ALU.add, None, ["modT"], ["modT"])
    ts("dve", modT[:, 32:40], modT[:, 32:40], 1.0, None, ALU.add, None, ["modT"], ["modT"])
    S.barrier()
    A.release(m0)

    def ln_stats(src, srckey, bufs):
        kp = bufs["key"]
        for cc in range(2):
            S.add("dve", lambda e, cc=cc: e.bn_stats(out=bufs["st"][:, cc, :], in_=src[:, cc * 512:(cc + 1) * 512]),
                  r=[srckey], w=[(kp, "st", cc)])
        S.add("dve", lambda e: e.bn_aggr(out=bufs["mv"][:], in_=bufs["st"][:].rearrange("p a b -> p (a b)")),
              r=[(kp, "st", 0), (kp, "st", 1)], w=[(kp, "mv")])
        ts("dve", bufs["ve"][:], bufs["mv"][:, 1:2], EPS, None, ALU.add, None, [(kp, "mv")], [(kp, "ve")])
        tt("pool", bufs["rs"][:], bufs["ve"][:], nhalf[:, 0:1], ALU.pow, [(kp, "ve"), "nhalf"], [(kp, "rs")])

    def ln_bufs(name):
        return {"st": A.alloc(name + "st", [P, 2, 6], F32), "mv": A.alloc(name + "mv", [P, 2], F32),
                "ve": A.alloc(name + "ve", [P, 1], F32), "rs": A.alloc(name + "rs", [P, 1], F32),
                "nm": A.alloc(name + "nm", [P, 1], F32), "key": name}

    m2 = A.mark()
    xt = [A.alloc("xt%d" % s, [P, D], F32) for s in range(2)]
    xn = [A.alloc("xn%d" % s, [P, D], BF16) for s in range(2)]
    lb = [ln_bufs("l1_%d" % s) for s in range(2)]
    pidx_i = A.alloc("pidxi", [P, 2], I32)
    pidx_f = A.alloc("pidxf", [P, 2], F32)
    qi_i = A.alloc("qii", [P, P], I32)
    qi_f = A.alloc("qif", [P, P], F32)
    A4 = A.alloc("A4", [P, 4], F32)
    KD = A.alloc("KD", [P, 4], F32)
    DTp = A.alloc("DTp", [P, 4, P], F32)
    QD = A.alloc("QD", [P, 4, P], F32)
    iota(pidx_i[:, 0:1], [[0, 1]], 1, 1, [("pidxi", 0)])
    iota(pidx_i[:, 1:2], [[0, 1]], 127, -1, [("pidxi", 1)])
    cp("dve", pidx_f[:], pidx_i[:], [("pidxi", 0), ("pidxi", 1)], ["pidxf"])
    iota(qi_i[:], [[1, P]], 1, 0, ["qii"])
    cp("dve", qi_f[:], qi_i[:], ["qii"], ["qif"])
    for h in range(4):
        act(A4[:, h:h + 1], pidx_f[:, 0:1], AF.Exp, ["pidxf", "lnsc"], [("A4", h)], bias=lnsc[:, 0:1], scale=-LG[h])
        act(KD[:, h:h + 1], pidx_f[:, 1:2], AF.Exp, ["pidxf", "lnsc"], [("KD", h)], bias=lnsc[:, 0:1], scale=LG[h])
        act(QD[:, h, :], qi_f[:], AF.Exp, ["qif"], [("QD", h)], scale=LG[h])
        ts("dve", DTp[:, h, :], cmask_f[:], A4[:, h:h + 1], None, ALU.mult, None, ["cmaskf", ("A4", h)], [("DTp", h)])
    CD = [math.exp(128.0 * LG[h]) for h in range(4)]

    rqdT = [A.alloc("rqdT%d" % s, [P, 4, 512], BF16) for s in range(2)]
    rkT = [A.alloc("rkT%d" % s, [P, 4, 512], BF16) for s in range(2)]
    rkd = [A.alloc("rkd%d" % s, [P, 4, 4, P], BF16) for s in range(2)]
    rv = [A.alloc("rv%d" % s, [P, 4, 512], BF16) for s in range(2)]
    rg1 = A.alloc("rg", [P, 4, 512], F32)
    PT = [A.alloc("PT%d" % s, [P, 4, P], BF16) for s in range(2)]
    ytmp = [A.alloc("ytmp%d" % s, [P, 4, P], F32) for s in range(2)]
    ygate = [A.alloc("ygate%d" % s, [P, 512], BF16) for s in range(2)]
    S32 = A.alloc("S32", [P, 4, P], F32)
    Sbf = [A.alloc("Sbf%d" % s, [P, 4, P], BF16) for s in range(2)]
    gst = [A.alloc("gst%d" % s, [P, 4, 6], F32) for s in range(2)]
    gmv = [A.alloc("gmv%d" % s, [P, 4, 2], F32) for s in range(2)]
    gve = [A.alloc("gve%d" % s, [P, 4], F32) for s in range(2)]
    grs = [A.alloc("grs%d" % s, [P, 4], F32) for s in range(2)]
    gnm = [A.alloc("gnm%d" % s, [P, 4], F32) for s in range(2)]

    def ln_part1(i):
        s2 = i % 2
        dma("sp", xt[s2][:], x[i * P:(i + 1) * P, :], [], ["xt%d" % s2], "xt%d" % s2)
        ln_stats(xt[s2], "xt%d" % s2, lb[s2])
        ts("dve", xn[s2][:], xt[s2][:], lb[s2]["mv"][:, 0:1], lb[s2]["rs"][:, 0:1], ALU.subtract, ALU.mult,
           ["xt%d" % s2, (lb[s2]["key"], "mv"), (lb[s2]["key"], "rs")], ["xn%d" % s2])

    def ln_part2(i):
        s2 = i % 2
        pve = pbf(6)[:, 0:512].rearrange("p (k t) -> p k t", k=4)
        pvo = pbf(7)[:, 512:1024].rearrange("p (k t) -> p k t", k=4)
        for k in range(KC):
            if k % 2 == 0:
                tr(pve[:, k // 2, :], xn[s2][:, k * P:(k + 1) * P], ident_b[:], ["xn%d" % s2, "identb"], [pbk[6]])
            else:
                tr(pvo[:, k // 2, :], xn[s2][:, k * P:(k + 1) * P], ident_b[:], ["xn%d" % s2, "identb"], [pbk[7]])
        for k in range(KC):
            dst = hT[:, k, i * P:(i + 1) * P]
            if k % 2 == 0:
                act(dst, pve[:, k // 2, :], AF.Identity, [pbk[6], "modT"], [("hT", i, k)],
                    bias=modT[:, k:k + 1], scale=modT[:, 8 + k:9 + k])
            else:
                ts("dve", dst, pvo[:, k // 2, :], modT[:, 8 + k:9 + k], modT[:, k:k + 1], ALU.mult, ALU.add,
                   [pbk[7], "modT"], [("hT", i, k)])

    def ln_tile(i):
        ln_part1(i)
        ln_part2(i)

    ucnt = {"fm": 0, "tm": 0}

    def proj_unit(g, u):
        gp = g % 2
        if u < 8:
            cb = u
            bk = ucnt["fm"] % 2
            ucnt["fm"] += 1
            for k in range(KC):
                mm(pb[bk][:], Wr[:, k, cb * P:(cb + 1) * P], hT[:, k, g * 512:(g + 1) * 512], k == 0, k == KC - 1,
                   [("Wr", cb // 4)] + [("hT", 4 * g + ti, k) for ti in range(4)], [pbk[bk]])
            if cb < 4:
                for c4 in range(4):
                    tt("dve", rqdT[gp][:, cb, c4 * P:(c4 + 1) * P], pb[bk][:, c4 * P:(c4 + 1) * P], QD[:, cb, :], ALU.mult,
                       [pbk[bk], ("QD", cb)], [("rqdT", gp, cb)])
            else:
                act(rkT[gp][:, cb - 4, :], pb[bk][:], AF.Identity, [pbk[bk]], [("rkT", gp, cb - 4)])
        else:
            ti, b3 = divmod(u - 8, 3)
            i = 4 * g + ti
            bk = 2 + (ucnt["tm"] % 2)
            ucnt["tm"] += 1
            off = 512 * (b3 + 1)
            for k in range(KC):
                mm(pb[bk][:], hT[:, k, i * P:(i + 1) * P], Wr[:, k, off:off + 512], k == 0, k == KC - 1,
                   [("Wr", b3 + 1), ("hT", i, k)], [pbk[bk]])
            if b3 == 0:
                for h in range(4):
                    act(rkd[gp][:, ti, h, :], pb[bk][:, h * P:(h + 1) * P], AF.Identity, [pbk[bk], ("KD", h)],
                        [("rkd", gp, ti, h)], scale=KD[:, h:h + 1])
            elif b3 == 1:
                cp("dve", rv[gp][:, ti, :], pb[bk][:], [pbk[bk]], [("rv", gp, ti)])
            else:
                act(rg1[:, ti, :], pb[bk][:], AF.Silu, [pbk[bk]], [("rg", ti)])

    def chunk_head(n):
        g, ti = divmod(n, 4)
        gp = g % 2
        cs = n % 2
        pS = pb[4][:].rearrange("p (h c) -> p h c", h=4)
        pY = pb[5][:].rearrange("p (h c) -> p h c", h=4)
        sl = slice(ti * P, (ti + 1) * P)
        for h in range(4):
            mm(pS[:, h, :], rkT[gp][:, h, sl], rqdT[gp][:, h, sl], True, True,
               [("rkT", gp, h), ("rqdT", gp, h)], [pbk[4]])
        tt("dve", PT[cs][:].rearrange("p h c -> p (h c)"), pb[4][:], DTp[:].rearrange("p h c -> p (h c)"), ALU.mult,
           [pbk[4]] + [("DTp", h) for h in range(4)], [("PT", cs)])
        yield
        for h in range(4):
            mm(pY[:, h, :], PT[cs][:, h, :], rv[gp][:, ti, h * P:(h + 1) * P], True, n == 0,
               [("PT", cs), ("rv", gp, ti)], [pbk[5]])
            if n > 0:
                mm(pY[:, h, :], rqdT[gp][:, h, sl], Sbf[(n - 1) % 2][:, h, :], False, True,
                   [("rqdT", gp, h), ("Sbf", (n - 1) % 2)], [pbk[5]])
        if n < NT - 1:
            pKV = pS
            for h in range(4):
                mm(pKV[:, h, :], rkd[gp][:, ti, h, :], rv[gp][:, ti, h * P:(h + 1) * P], True, True,
                   [("rkd", gp, ti, h), ("rv", gp, ti)], [pbk[4]])
            if n == 0:
                cp("dve", S32[:].rearrange("p h c -> p (h c)"), pb[4][:], [pbk[4]], ["S32"])
            else:
                for h in range(4):
                    stt(S32[:, h, :], S32[:, h, :], CD[h], pKV[:, h, :], ALU.mult, ALU.add, ["S32", pbk[4]], ["S32"])
            act(Sbf[cs][:].rearrange("p h c -> p (h c)"), S32[:].rearrange("p h c -> p (h c)"), AF.Identity, ["S32"], [("Sbf", cs)])
        for h in range(4):
            S.add("dve", lambda e, h=h, pY=pY, cs=cs: e.bn_stats(out=gst[cs][:, h, :], in_=pY[:, h, :]),
                  r=[pbk[5]], w=[("gst", cs, h)])
        for h in range(4):
            S.add("dve", lambda e, h=h, cs=cs: e.bn_aggr(out=gmv[cs][:, h, :], in_=gst[cs][:, h, :]),
                  r=[("gst", cs, h)], w=[("gmv", cs, h)])
        ts("dve", gve[cs][:], gmv[cs][:, :, 1], EPS, None, ALU.add, None, [("gmv", cs, h) for h in range(4)], [("gve", cs)])
        tt("pool", grs[cs][:], gve[cs][:], nhalf[:, 0:4], ALU.pow, [("gve", cs), "nhalf"], [("grs", cs)])
        yield
        stt(gnm[cs][:], gmv[cs][:, :, 0], -1.0, grs[cs][:], ALU.mult, ALU.mult,
            [("gmv", cs, h) for h in range(4)] + [("grs", cs)], [("gnm", cs)])
        for h in range(4):
            act(ytmp[cs][:, h, :], pY[:, h, :], AF.Identity, [pbk[5], ("grs", cs), ("gnm", cs)], [("ytmp", cs, h)],
                bias=gnm[cs][:, h:h + 1], scale=grs[cs][:, h:h + 1])
        yield

    def tail_gate(n):
        g, ti = divmod(n, 4)
        cs = n % 2
        tt("dve", ygate[cs][:], ytmp[cs][:].rearrange("p h c -> p (h c)"), rg1[:, ti, :], ALU.mult,
           [("ytmp", cs, h) for h in range(4)] + [("rg", ti)], [("ygate", cs)])

    def tail_tr(n):
        cs = n % 2
        pTr = pbf(7)[:, 0:512].rearrange("p (h c) -> p h c", h=4)
        for h in range(4):
            tr(pTr[:, h, :], ygate[cs][:, h * P:(h + 1) * P], ident_b[:], [("ygate", cs), "identb"], [pbk[7]])
        cp("dve", yTr[:, :, n * P:(n + 1) * P], pTr, [pbk[7]], [("yTr", n)])

    LNA = 9
    for i in range(4):
        ln_tile(i)
    for u in range(20):
        proj_unit(0, u)
        if u % 4 == 3:
            ln_tile(4 + u // 4)
    ln_pending = None
    for n in range(NT):
        g, ti = divmod(n, 4)
        units = [(g + 1, u) for u in (2 * ti, 2 * ti + 1, 8 + 3 * ti, 9 + 3 * ti)] if g + 1 < NG else []
        gen = chunk_head(n)
        next(gen)
        for (gg, u) in units[0:2]:
            proj_unit(gg, u)
        next(gen)
        if n > 0:
            tail_gate(n - 1)
        for (gg, u) in units[2:4]:
            proj_unit(gg, u)
        if n > 0:
            tail_tr(n - 1)
            g1_, t1_ = divmod(n - 1, 4)
            if g1_ + 1 < NG:
                proj_unit(g1_ + 1, 10 + 3 * t1_)
        if ln_pending is not None:
            ln_part2(ln_pending)
            ln_pending = None
        next(gen)
        if n + LNA < NT:
            ln_part1(n + LNA)
            ln_pending = n + LNA
        cast_ffn_piece(n)
    tail_gate(NT - 1)
    tail_tr(NT - 1)
    assert len(cast_jobs) <= NT
    S.barrier()
    A.release(m2)

    for f in range(FC):
        for hf in range(2):
            S.add("sp", lambda e, f=f, hf=hf: e.dma_start(
                out=wfi_r[f].rearrange("p (k c) -> p k c", k=KC)[:, :, hf * P:(hf + 1) * P],
                in_=wfi_sv[:, :, hf * DFF + f * P:hf * DFF + (f + 1) * P]), r=["wfis"], w=["wfir"], dma="wfir", bg=True)
    m3 = A.mark()
    Wm = A.alloc("Wm", [P, KC, 1536], BF16)
    for cbk in range(3):
        dma("pool", Wm[:, :, cbk * 512:(cbk + 1) * 512], w_in_v[:, :, 2048 + cbk * 512:2048 + (cbk + 1) * 512], [],
            [("Wm", cbk)], "Wm%d" % cbk)
    qT = A.alloc("qT", [P, T], BF16)
    kT = A.alloc("kT", [P, T], BF16)
    Va = A.alloc("Va", [P, NT, 129], BF16)
    ksum = A.alloc("ksum", [P, 16], F32)
    ksb = A.alloc("ksb", [P, 16], BF16)
    gmask = A.alloc("gmask", [P, NT, 16], F32)
    gsb = A.alloc("gsb", [P, NT, 16], F32)
    m8 = A.alloc("m8", [P, NT, 8], F32)
    thr = A.alloc("thr", [P, NT], F32)
    sel = A.alloc("sel", [P, NT, 16], F32)
    iot_i = A.alloc("ioti", [P, 32], I32)
    IOT = A.alloc("IOT", [P, 32], F32)
    AB = A.alloc("AB", [P, 32], F32)
    acc = [A.alloc("acc%d" % s, [P, 4, 129], F32) for s in range(2)]
    PTm = [A.alloc("PTm%d" % s, [P, 512], BF16) for s in range(6)]
    rinv = [A.alloc("rinv%d" % s, [P, 1], F32) for s in range(4)]
    ymo = [A.alloc("ymo%d" % s, [P, P], BF16) for s in range(4)]

    ms("dve", Va[:, :, 128:129], 1.0, ["Va1"])
    ms("pool", gmask[:], 0.0, ["gmask"])
    asel(gmask[:].rearrange("p i n -> p (i n)"), gmask[:].rearrange("p i n -> p (i n)"), [[1, 16], [0, 2], [-1, 16]],
         ALU.is_ge, NEG, -1, 0, ["gmask"], ["gmask"])
    iota(iot_i[:], [[128, 32]], -28 * 128 - 256, 1, ["ioti"])
    cp("dve", IOT[:], iot_i[:], ["ioti"], ["IOT"])

    def stage4_prologue():
        for e_ in ("act", "dve", "pool", "sp"):
            S.add(e_, lambda h: h.nop(), w=["hTall"])
        Wo = A_hT.alloc("Wo", [P, KC, D], BF16)
        for cbk in range(2):
            dma("pool", Wo[:, :, cbk * 512:(cbk + 1) * 512], w_out.rearrange("(k p) n -> p k n", p=P)[:, :, cbk * 512:(cbk + 1) * 512],
                [], [("Wo", cbk)], "Wo%d" % cbk)
        lnT = A_hT.alloc("lnT", [P, 4, D], F32)
        g2bc = A_hT.alloc("g2bc", [P, D], F32)
        tmp2 = [A_hT.alloc("tmp2_%d" % s, [P, 512], F32) for s in range(2)]
        actT = A_hT.alloc("actT", [P, FC, 512], BF16)
        A_pro = Arena(nc, A_hT.top - FC * 512 * 2, A_hT.top)
        lnrow = A_pro.alloc("lnrow", [1, 1, D], F32)
        g1bc = A_pro.alloc("g1bc", [P, D], F32)
        Gt = [A_pro.alloc("Gt%d" % s, [P, P], F32) for s in range(2)]
        bc = 0
        for q, src in enumerate((ln1_w, ln1_b, ln2_w, ln2_b)):
            dma("sp", lnrow[0:1, 0, :], src, [], ["lnrow"], "lnrow")
            for cbk in range(2):
                bk = bc % 4
                bc += 1
                mm(pb[bk][:], ones_f[0:1, 0:P], lnrow[0:1, 0, cbk * 512:(cbk + 1) * 512], True, True, ["lnrow", "onesf"], [pbk[bk]])
                cp("dve", lnT[:, q, cbk * 512:(cbk + 1) * 512], pb[bk][:], [pbk[bk]], [("lnT", q)])
        for q, (base, dstt) in enumerate(((16, g1bc), (40, g2bc))):
            for cbk in range(2):
                bk = bc % 4
                bc += 1
                for jj in range(4):
                    j = cbk * 4 + jj
                    gs = (q * 8 + j) % 2
                    ts("dve", Gt[gs][:], ones_f[:], modT[:, base + j:base + j + 1], None, ALU.mult, None, ["onesf", "modT"], [("Gt", gs)])
                    mm(pb[bk][:, jj * P:(jj + 1) * P], Gt[gs][:], ident_f[:], True, True, [("Gt", gs), "identf"], [pbk[bk]])
                cp("dve", dstt[:, cbk * 512:(cbk + 1) * 512], pb[bk][:], [pbk[bk]], [("gbc", q)])
        for k in range(KC):
            tt("dve", Wo[:, k, :], Wo[:, k, :], g1bc[:], ALU.mult, [("Wo", 0), ("Wo", 1), ("gbc", 0)], [("Wof", k)])
        return Wo, lnT, g2bc, tmp2, actT

    mst = {"ocnt": 0, "ptc": 0, "trc": 0}
    NPT = 6
    for hm in range(4):
        hkey = lambda g: [("hT", 4 * g + ti, k) for ti in range(4) for k in range(KC)]
        for g in range(NG):
            bk = g % 2
            for k in range(KC):
                mm(pb[bk][:], Wm[:, k, 512 + hm * P:512 + (hm + 1) * P], hT[:, k, g * 512:(g + 1) * 512], k == 0, k == KC - 1,
                   [("Wm", 1), "hTall"], [pbk[bk]])
            act(kT[:, g * 512:(g + 1) * 512], pb[bk][:], AF.Identity, [pbk[bk]], [("kT", g), ("bser", bk)])
            S.add("dve", lambda e, bk=bk, g=g: e.tensor_reduce(out=ksum[:, 2 * g:2 * g + 2],
                                                               in_=pb[bk][:].rearrange("p (b s) -> p b s", b=2),
                                                               axis=AX.X, op=ALU.add),
                  r=[pbk[bk]], w=[("ksum", g), ("bser", bk)])
        cp("dve", ksb[:], ksum[:], [("ksum", g) for g in range(NG)], ["ksb"])
        for g in range(NG):
            bk = g % 2
            for k in range(KC):
                mm(pb[bk][:], Wm[:, k, hm * P:(hm + 1) * P], hT[:, k, g * 512:(g + 1) * 512], k == 0, k == KC - 1,
                   [("Wm", 0), "hTall"], [pbk[bk]])
            act(qT[:, g * 512:(g + 1) * 512], pb[bk][:], AF.Identity, [pbk[bk]], [("qT", g)])
        pg = pb[2][:].rearrange("p (i n) -> p i n", n=16)
        for i in range(NT):
            mm(pg[:, i, :], qT[:, i * P:(i + 1) * P], ksb[:], True, True, [("qT", i // 4), "ksb"], [pbk[2]])
        tt("dve", gsb[:].rearrange("p i n -> p (i n)"), pb[2][:], gmask[:].rearrange("p i n -> p (i n)"), ALU.add,
           [pbk[2], "gmask"], ["gsb"])
        for i in range(2, NT):
            S.add("dve", lambda e, i=i: e.max(out=m8[:, i, :], in_=gsb[:, i, :]), r=["gsb"], w=[("m8", i)])
        ts("dve", thr[:, 2:NT], m8[:, 2:NT, 2], -1.0e29, None, ALU.max, None, [("m8", i) for i in range(2, NT)], ["thr"])
        for i in range(2, NT):
            ts("dve", sel[:, i, :], gsb[:, i, :], thr[:, i:i + 1], None, ALU.is_ge, None, ["gsb", "thr"], [("sel", i)])
        for i in range(NT):
            bk = 6 + (i % 2)
            for k in range(KC):
                mm(pb[bk][:, 0:P], hT[:, k, i * P:(i + 1) * P], Wm[:, k, 1024 + hm * P:1024 + (hm + 1) * P], k == 0, k == KC - 1,
                   [("Wm", 2), "hTall"], [pbk[bk]])
            act(Va[:, i, 0:P], pb[bk][:, 0:P], AF.Identity, [pbk[bk]], [("Va", i)])
        ts("dve", AB[:], IOT[:], SLOPES[hm], None, ALU.mult, None, ["IOT"], ["AB"])
        def phase1(gq, n, st):
            qts = [ti for ti in range(4) if (2 * gq + ti // 2) >= n]
            pts = {}
            for kt in (2 * n, 2 * n + 1):
                qk = [ti for ti in qts if 4 * gq + ti >= kt]
                if not qk:
                    continue
                c0 = min(qk) * P
                sb = (0, 1, 6, 7)[st["ptc"] % 4]
                ptb = PTm[st["ptc"] % NPT]
                pkey = ("PTm", st["ptc"] % NPT)
                st["ptc"] += 1
                pts[kt] = (ptb, pkey, qk)
                mm(pb[sb][:, c0:512], kT[:, kt * P:(kt + 1) * P], qT[:, gq * 512 + c0:(gq + 1) * 512], True, True,
                   [("kT", kt // 4), ("qT", gq)], [pbk[sb]])
                rel = kt - 4 * gq + 28
                act(ptb[:, c0:512], pb[sb][:, c0:512], AF.Exp, [pbk[sb], "AB"], [pkey], bias=AB[:, rel:rel + 1], scale=SCALE)
                if kt >= 4 * gq:
                    td = kt - 4 * gq
                    tt("dve", ptb[:, td * P:(td + 1) * P], ptb[:, td * P:(td + 1) * P], cmask_b[:], ALU.mult,
                       [pkey, "cmaskb"], [pkey])
            return qts, pts

        def phase2(gq, n, qts, pts, st):
            accs = acc[gq % 2]
            ak = gq % 2
            oset = st["ocnt"] % 2
            st["ocnt"] += 1
            banks = (2 + 2 * oset, 3 + 2 * oset)
            for ti in qts:
                bank = banks[ti // 2]
                reg = pb[bank][:, (ti % 2) * 129:(ti % 2) * 129 + 129]
                kts = [kt for kt in (2 * n, 2 * n + 1) if kt in pts and ti in pts[kt][2]]
                for kt in kts:
                    ptb, pkey, _ = pts[kt]
                    mm(reg, ptb[:, ti * P:(ti + 1) * P], Va[:, kt, :], kt == kts[0], kt == kts[-1],
                       [pkey, ("Va", kt), "Va1"], [pbk[bank]])
            for ti in qts:
                qi = 4 * gq + ti
                bank = banks[ti // 2]
                reg = pb[bank][:, (ti % 2) * 129:(ti % 2) * 129 + 129]
                own = (n == qi // 2)
                sc = 1.0 if own else sel[:, qi, n:n + 1]
                rds = [pbk[bank]] + ([] if own else [("sel", qi)])
                if n == 0:
                    ts("dve", accs[:, ti, :], reg, sc, None, ALU.mult, None, rds, [("acc", ak, ti)])
                else:
                    stt(accs[:, ti, :], reg, sc, accs[:, ti, :], ALU.mult, ALU.add, rds + [("acc", ak, ti)], [("acc", ak, ti)])

        def fin_dve(gq):
            accs = acc[gq % 2]
            ak = gq % 2
            for ti in range(4):
                S.add("dve", lambda e, ti=ti, accs=accs: e.reciprocal(out=rinv[ti][:], in_=accs[:, ti, 128:129]),
                      r=[("acc", ak, ti)], w=[("rinv", ti)])
                ts("dve", ymo[ti][:], accs[:, ti, 0:P], rinv[ti][:, 0:1], None, ALU.mult, None,
                   [("acc", ak, ti), ("rinv", ti)], [("ymo", ti)])

        def fin_pe(gq):
            tb = (0, 1, 6, 7)[mst["ptc"] % 4]
            mst["ptc"] += 1
            for ti in range(4):
                qi = 4 * gq + ti
                pT_ = pbf(tb)[:, ti * P:(ti + 1) * P]
                tr(pT_, ymo[ti][:], ident_b[:], [("ymo", ti), "identb"], [pbk[tb]])
            for ti in range(4):
                qi = 4 * gq + ti
                pT_ = pbf(tb)[:, ti * P:(ti + 1) * P]
                cp("dve", yTm[:, hm, qi * P:(qi + 1) * P], pT_, [pbk[tb]], [("yTm", hm, qi)])

        blocks = [(gq, n) for gq in range(NG) for n in range(2 * gq + 2)]
        pend = None
        fin_pending = None
        for (gq, n) in blocks:
            qts, pts = phase1(gq, n, mst)
            if pend is not None:
                phase2(*pend, mst)
                if fin_pending is not None:
                    fin_pe(fin_pending)
                    fin_pending = None
                if pend[1] == 2 * pend[0] + 1:
                    fin_dve(pend[0])
                    fin_pending = pend[0]
            pend = (gq, n, qts, pts)
        phase2(*pend, mst)
        if fin_pending is not None:
            fin_pe(fin_pending)
        fin_dve(pend[0])
        fin_pe(pend[0])
    S.barrier()
    A.release(m3)

    Wo, lnT, g2bc, tmp2, actT = stage4_prologue()
    S.barrier()
    x1 = [[A.alloc("x1_%d_%d" % (gp, s), [P, D], F32) for s in range(4)] for gp in range(2)]
    xn2 = [A.alloc("xn2_%d" % s, [P, D], BF16) for s in range(4)]
    h2T = A.alloc("h2T", [P, KC, 512], BF16)
    sg = [A.alloc("sg%d" % s, [P, 512], F32) for s in range(2)]
    NWI = 3
    NWO = 4
    wfi = [A.alloc("wfi%d" % s, [P, KC, 256], BF16) for s in range(3)]
    wfo = [A.alloc("wfo%d" % s, [P, D], BF16) for s in range(NWO)]
    lf = [ln_bufs("lf%d" % s) for s in range(4)]
    lc = [ln_bufs("lc%d" % s) for s in range(4)]

    def yT_sl(hh, i):
        if hh < 4:
            return yTr[:, hh, i * P:(i + 1) * P]
        return yTm[:, hh - 4, i * P:(i + 1) * P]

    def ln_finish(buf, key, lb):
        kp = lb["key"]
        stt(lb["nm"][:], lb["mv"][:, 0:1], -1.0, lb["rs"][:], ALU.mult, ALU.mult, [(kp, "mv"), (kp, "rs")], [(kp, "nm")])

    def ln_norm(buf, key, lb):
        kp = lb["key"]
        act(buf[:], buf[:], AF.Identity, [key, (kp, "rs"), (kp, "nm")], [key], bias=lb["nm"][:, 0:1], scale=lb["rs"][:, 0:1])

    def ln_affine(buf, key, wq):
        tt("dve", buf[:], buf[:], lnT[:, wq, :], ALU.mult, [key, ("lnT", wq)], [key])
        tt("dve", buf[:], buf[:], lnT[:, wq + 1, :], ALU.add, [key, ("lnT", wq + 1)], [key])

    def front_a(g):
        gp = g % 2
        for ti in range(4):
            i = 4 * g + ti
            dma("sp", x1[gp][ti][:], x[i * P:(i + 1) * P, :], [], [("x1", gp, ti)], "xld%d_%d" % (gp, ti))
        for ti in range(4):
            i = 4 * g + ti
            banks = (2 * (ti % 2), 2 * (ti % 2) + 1)
            for cbk in range(2):
                for hh in range(8):
                    mm(pb[banks[cbk]][:], yT_sl(hh, i), Wo[:, hh, cbk * 512:(cbk + 1) * 512], hh == 0, hh == 7,
                       [("Wof", hh)], [pbk[banks[cbk]]])
            for cbk in range(2):
                hs = slice(cbk * 512, (cbk + 1) * 512)
                stt(x1[gp][ti][:, hs], x1[gp][ti][:, hs], ALPHA, pb[banks[cbk]][:], ALU.mult, ALU.add,
                    [("x1", gp, ti), pbk[banks[cbk]]], [("x1", gp, ti)])
            ln_stats(x1[gp][ti], ("x1", gp, ti), lf[ti])
        for ti in range(4):
            ln_finish(x1[gp][ti], ("x1", gp, ti), lf[ti])
        for ti in range(4):
            ln_norm(x1[gp][ti], ("x1", gp, ti), lf[ti])
        for ti in range(4):
            ln_affine(x1[gp][ti], ("x1", gp, ti), 0)
            ln_stats(x1[gp][ti], ("x1", gp, ti), lf[ti])
        for ti in range(4):
            ts("dve", xn2[ti][:], x1[gp][ti][:], lf[ti]["mv"][:, 0:1], lf[ti]["rs"][:, 0:1], ALU.subtract, ALU.mult,
               [("x1", gp, ti), (lf[ti]["key"], "mv"), (lf[ti]["key"], "rs")], [("xn2", ti)])

    def front_b(g):
        for ti in range(4):
            bke, bko = 4 + 2 * (ti % 2), 5 + 2 * (ti % 2)
            pve = pbf(bke).rearrange("p (k t) -> p k t", k=KC)
            pvo = pbf(bko).rearrange("p (k t) -> p k t", k=KC)
            for k in range(KC):
                if k % 2 == 0:
                    tr(pve[:, k // 2, :], xn2[ti][:, k * P:(k + 1) * P], ident_b[:], [("xn2", ti), "identb"], [pbk[bke]])
                else:
                    tr(pvo[:, k // 2, :], xn2[ti][:, k * P:(k + 1) * P], ident_b[:], [("xn2", ti), "identb"], [pbk[bko]])
            for k in range(KC):
                dst = h2T[:, k, ti * P:(ti + 1) * P]
                if k % 2 == 0:
                    act(dst, pve[:, k // 2, :], AF.Identity, [pbk[bke], "modT"], [("h2T", ti, k)],
                        bias=modT[:, 24 + k:25 + k], scale=modT[:, 32 + k:33 + k])
                else:
                    ts("dve", dst, pvo[:, k // 2, :], modT[:, 32 + k:33 + k], modT[:, 24 + k:25 + k], ALU.mult, ALU.add,
                       [pbk[bko], "modT"], [("h2T", ti, k)])

    cnt = {"wi": 0, "wo": 0}

    def ffn_in(g, pending):
        for f in range(FC):
            if pending:
                pending.pop(0)()
            sl_ = cnt["wi"] % NWI
            cnt["wi"] += 1
            dma("sp", wfi[sl_][:].rearrange("p k c -> p (k c)"), wfi_r[f], ["wfir"], [("wfi", sl_)], "wfi%d" % sl_)
            gb, ub = 4 + 2 * (f % 2), 5 + 2 * (f % 2)
            for half, bk in ((0, gb), (1, ub)):
                for k in range(KC):
                    mm(pb[bk][:], wfi[sl_][:, k, half * P:(half + 1) * P], h2T[:, k, :], k == 0, k == KC - 1,
                       [("wfi", sl_)] + [("h2T", ti, k) for ti in range(4)], [pbk[bk]])
            act(sg[f % 2][:], pb[gb][:], AF.Silu, [pbk[gb]], [("sg", f % 2)])
            tt("dve", actT[:, f, :], sg[f % 2][:], pb[ub][:], ALU.mult, [("sg", f % 2), pbk[ub]], [("actT", f)])

    def ffn_out(g):
        for f in range(FC):
            sl_ = cnt["wo"] % NWO
            cnt["wo"] += 1
            dma("sp", wfo[sl_][:], wfo_s[f * P:(f + 1) * P, :], ["wfos"], [("wfo", sl_)], "wfo%d" % sl_)
            for ti in range(4):
                for cbk in range(2):
                    bk = ti * 2 + cbk
                    mm(pb[bk][:], actT[:, f, ti * P:(ti + 1) * P], wfo[sl_][:, cbk * 512:(cbk + 1) * 512], f == 0, f == FC - 1,
                       [("actT", f), ("wfo", sl_)], [pbk[bk]])

    def tail_a1(g):
        gp = g % 2
        tc = 0
        for ti in (2, 3, 0, 1):
            for cbk in range(2):
                hs = slice(cbk * 512, (cbk + 1) * 512)
                bk = ti * 2 + cbk
                t2 = tmp2[tc % 2]
                tk = ("tmp2", tc % 2)
                tc += 1
                tt("dve", t2[:], pb[bk][:], g2bc[:, hs], ALU.mult, [pbk[bk], ("gbc", 1)], [tk])
                stt(x1[gp][ti][:, hs], x1[gp][ti][:, hs], ALPHA, t2[:], ALU.mult, ALU.add, [("x1", gp, ti), tk], [("x1", gp, ti)])

    def tail_items(g):
        gp = g % 2
        items = []
        for ti in range(4):
            items.append(lambda ti=ti: ln_stats(x1[gp][ti], ("x1", gp, ti), lc[ti]))
        for ti in range(4):
            items.append(lambda ti=ti: ln_finish(x1[gp][ti], ("x1", gp, ti), lc[ti]))
        for ti in range(4):
            items.append(lambda ti=ti: ln_norm(x1[gp][ti], ("x1", gp, ti), lc[ti]))
        for ti in range(4):
            items.append(lambda ti=ti: tt("dve", x1[gp][ti][:], x1[gp][ti][:], lnT[:, 2, :], ALU.mult,
                                          [("x1", gp, ti), ("lnT", 2)], [("x1", gp, ti)]))
        for ti in range(4):
            def fin(ti=ti):
                tt("dve", x1[gp][ti][:], x1[gp][ti][:], lnT[:, 3, :], ALU.add, [("x1", gp, ti), ("lnT", 3)], [("x1", gp, ti)])
                i = 4 * g + ti
                S.wait_at_end(dma("pool", y[i * P:(i + 1) * P, :], x1[gp][ti][:], [("x1", gp, ti)], [("y", i)], "yout%d_%d" % (gp, ti)))
            items.append(fin)
        return items

    front_a(0)
    front_b(0)
    pending = []
    for g in range(NG):
        ffn_in(g, pending)
        assert not pending
        if g + 1 < NG:
            front_a(g + 1)
        ffn_out(g)
        tail_a1(g)
        if g + 1 < NG:
            front_b(g + 1)
        pending = tail_items(g)
    for it in pending:
        it()
    S.emit()
    return nc


_NC_CACHE = {}


def kernel(x, c, w_ada, b_ada, w_in, w_out, ln1_w, ln1_b, w_ffn_in, w_ffn_out, ln2_w, ln2_b):
    f = lambda a: np.ascontiguousarray(np.asarray(a, dtype=np.float32))
    x = f(x)
    c = f(c)
    B = x.shape[0]
    if "nc" not in _NC_CACHE:
        _NC_CACHE["nc"] = build_nc()
    nc = _NC_CACHE["nc"]
    shared = {
        "w_ada": f(w_ada)[0], "b_ada": f(b_ada)[0:1], "w_in": f(w_in)[0], "w_out": f(w_out)[0],
        "ln1_w": f(ln1_w)[0:1], "ln1_b": f(ln1_b)[0:1], "w_ffn_in": f(w_ffn_in)[0], "w_ffn_out": f(w_ffn_out)[0],
        "ln2_w": f(ln2_w)[0:1], "ln2_b": f(ln2_b)[0:1],
    }
    in_maps = []
    for b in range(B):
        m = dict(shared)
        m["x"] = x[b]
        m["c"] = c[b:b + 1]
        in_maps.append(m)
    res = run_bass_kernel_spmd(nc, in_maps, core_ids=list(range(B)))
    return np.stack([np.asarray(r["y"], dtype=np.float32) for r in res.results], axis=0)
```
